# Optimizing a Trainium2 kernel written in Bass

```python
import math
import jax, jax.numpy as jnp
from jax import lax
import numpy as np

D_MODEL = 1024
BATCH = 8
SEQ = 4096
DEPTH = 4

GRID_W = 64
CTX_LEN = 256
NORM_EPS = 1e-6
ROPE_THETA = 10000.0
Q_BLOCK = 128

HEAD_DIM = 64
GQA_HEADS = 4
GQA_KV_HEADS = 2
GMLP_GROUPS = 4
GMLP_CH = 64
GMLP_CHUNK = 128
DIFF_HEADS = 4
DIFF_DIM = 32
DN_HEADS = 4
DN_DK = 64
DN_DV = 64
DN_CONV = 3
DN_CHUNK = 64
N_EXPERTS = 32
TOP_K = 4
D_EXPERT = D_MODEL
SWIGLU_LIMIT = 7.0
SWIGLU_ALPHA = 1.702
MOE_BLOCK = 128

GQA_W = GQA_HEADS * HEAD_DIM
GMLP_W = GMLP_GROUPS * GMLP_CH
DIFF_W = DIFF_HEADS * 2 * DIFF_DIM
DN_W = DN_HEADS * DN_DV
DN_QKV = DN_HEADS * (2 * DN_DK + DN_DV)
MIX_WIDTH = GQA_W + GMLP_W + DIFF_W + DN_W
IN_SIZES = (GQA_W, GQA_KV_HEADS * HEAD_DIM, GQA_KV_HEADS * HEAD_DIM,
            GMLP_W, GMLP_W,
            DIFF_W, DIFF_W, DIFF_W,
            DN_QKV, DN_W, 2 * DN_HEADS, 2 * DN_HEADS)
D_IN = sum(IN_SIZES)
IN_OFFSETS = tuple(int(o) for o in np.cumsum(IN_SIZES)[:-1])

kernel_name = 'hybrid_head_group_diffusion_trunk'


def rms_norm(x, g):
    xf = x.astype(jnp.float32)
    y = xf * lax.rsqrt(jnp.mean(xf * xf, axis=-1, keepdims=True) + NORM_EPS)
    return (y * g.astype(jnp.float32)).astype(x.dtype)


def l2_norm(x):
    xf = x.astype(jnp.float32)
    return (xf * lax.rsqrt(jnp.sum(xf * xf, axis=-1, keepdims=True) + NORM_EPS)).astype(x.dtype)


def modulate(h, shift, scale):
    return h * (1.0 + scale) + shift


def heads(t, n):
    return t.reshape(*t.shape[:-1], n, t.shape[-1] // n)


def flip_t(t):
    return jnp.flip(t, axis=1)


def axial_rope(row_pos, col_pos, dim):
    n = dim // 4
    inv = jnp.power(ROPE_THETA, -jnp.arange(n, dtype=jnp.float32) / n)
    ang = jnp.concatenate([row_pos[:, None] * inv, col_pos[:, None] * inv], axis=-1)
    return jnp.cos(ang), jnp.sin(ang)


def apply_rope(x, rope):
    cos, sin = rope
    half = x.shape[-1] // 2
    x1, x2 = x[..., :half], x[..., half:]
    cs, sn = cos[:, None, :], sin[:, None, :]
    return jnp.concatenate([x1 * cs - x2 * sn, x1 * sn + x2 * cs], axis=-1).astype(x.dtype)


def sweep_query_blocks(fn, *qs):
    b, l = qs[0].shape[:2]
    nb = l // Q_BLOCK
    blocks = tuple(q.reshape(b, nb, Q_BLOCK, *q.shape[2:]).swapaxes(0, 1) for q in qs)
    out = lax.map(lambda args: fn(*args), blocks)
    return out.swapaxes(0, 1).reshape(b, l, *out.shape[3:])


def gqa_attend(q, k, v):
    scale = HEAD_DIM ** -0.5
    def one_block(qb):
        s = jnp.einsum('bqhgd,bkhd->bhgqk', qb, k).astype(jnp.float32) * scale
        p = jax.nn.softmax(s, axis=-1).astype(v.dtype)
        return jnp.einsum('bhgqk,bkhd->bqhgd', p, v)
    return sweep_query_blocks(one_block, q)


def diff_attend(q1, q2, k1, k2, v, lam):
    scale = DIFF_DIM ** -0.5
    def one_block(q1b, q2b):
        p1 = jax.nn.softmax(jnp.einsum('bqhd,bkhd->bhqk', q1b, k1).astype(jnp.float32) * scale, axis=-1)
        p2 = jax.nn.softmax(jnp.einsum('bqhd,bkhd->bhqk', q2b, k2).astype(jnp.float32) * scale, axis=-1)
        return jnp.einsum('bhqk,bkhd->bqhd', (p1 - lam * p2).astype(v.dtype), v)
    return sweep_query_blocks(one_block, q1, q2)


def gmlp_spatial(u, v, w_s, b_s, g_v):
    b, l, _ = v.shape
    vn = rms_norm(v, g_v).reshape(b, l // GMLP_CHUNK, GMLP_CHUNK, GMLP_GROUPS, GMLP_CH)
    sp = jnp.einsum('gij,bnjgc->bnigc', w_s, vn) + b_s.T[None, None, :, :, None]
    return u * sp.reshape(b, l, GMLP_W)


def short_conv(x, w):
    ch = x.shape[-1]
    y = lax.conv_general_dilated(x, w[:, None, :].astype(x.dtype), window_strides=(1,),
                                 padding=[(DN_CONV // 2, DN_CONV // 2)],
                                 dimension_numbers=('NWC', 'WIO', 'NWC'), feature_group_count=ch)
    return jax.nn.silu(y)


def gated_delta_chunked(q, k, v, beta, g, s0):
    b, l, h, dk = q.shape
    dv = v.shape[-1]
    n = l // DN_CHUNK

    def chunked(t):
        t = t.astype(jnp.float32).reshape(b, n, DN_CHUNK, h, *t.shape[3:])
        return jnp.moveaxis(t, 3, 1)

    qc, kc, vc, bc, gc = (chunked(t) for t in (q, k, v, beta, g))
    cum_g = jnp.cumsum(gc, axis=-1)
    idx = jnp.arange(DN_CHUNK)
    incl = idx[:, None] >= idx[None, :]
    strict = idx[:, None] > idx[None, :]
    decay = jnp.exp(jnp.where(incl, cum_g[..., :, None] - cum_g[..., None, :], -jnp.inf))
    a_mat = jnp.where(strict, jnp.einsum('bhnid,bhnjd->bhnij', kc, kc) * decay * bc[..., :, None], 0.0)
    t_mat = a_mat + jnp.eye(DN_CHUNK, dtype=jnp.float32)
    rhs = jnp.concatenate([bc[..., None] * vc, (bc * jnp.exp(cum_g))[..., None] * kc], axis=-1)
    sol = lax.linalg.triangular_solve(t_mat, rhs, left_side=True, lower=True, unit_diagonal=True)
    u_mat, w_mat = sol[..., :dv], sol[..., dv:]
    qk = jnp.einsum('bhnid,bhnjd->bhnij', qc, kc) * decay
    xs = tuple(jnp.moveaxis(t, 2, 0) for t in (qc, kc, u_mat, w_mat, cum_g, qk))

    def step(s, inp):
        q_, k_, u_, w_, g_, qk_ = inp
        new_v = u_ - jnp.einsum('bhck,bhvk->bhcv', w_, s)
        o = jnp.exp(g_)[..., None] * jnp.einsum('bhck,bhvk->bhcv', q_, s) + jnp.einsum('bhij,bhjv->bhiv', qk_, new_v)
        g_end = g_[..., -1:]
        s = jnp.exp(g_end)[..., None] * s + jnp.einsum('bhcv,bhck->bhvk', new_v * jnp.exp(g_end - g_)[..., None], k_)
        return s, o

    s_fin, o = lax.scan(step, s0.astype(jnp.float32), xs)
    o = o.transpose(1, 0, 3, 2, 4).reshape(b, l, h, dv)
    return o.astype(v.dtype), s_fin


def gdn_inputs(qkv_raw, b_raw, a_raw, conv_w, a_log, dt_bias):
    b, l, _ = qkv_raw.shape
    qkv = short_conv(qkv_raw, conv_w)
    q, k, v = jnp.split(qkv, [DN_HEADS * DN_DK, 2 * DN_HEADS * DN_DK], axis=-1)
    q = l2_norm(heads(q, DN_HEADS)) * (DN_DK ** -0.5)
    k = l2_norm(heads(k, DN_HEADS))
    v = heads(v, DN_HEADS)
    beta = jax.nn.sigmoid(b_raw.astype(jnp.float32)).reshape(b, l, 2, DN_HEADS)
    g = -jnp.exp(a_log) * jax.nn.softplus(a_raw.astype(jnp.float32).reshape(b, l, 2, DN_HEADS) + dt_bias)
    return q, k, v, beta, g


def gdn_bidirectional(ctx_in, lat_in, need_ctx):
    qc, kc, vc, bc, gc = ctx_in
    ql, kl, vl, bl, gl = lat_in
    s0 = jnp.zeros((qc.shape[0], DN_HEADS, DN_DV, DN_DK), jnp.float32)
    o_cf, s_f = gated_delta_chunked(qc, kc, vc, bc[:, :, 0], gc[:, :, 0], s0)
    o_cb, s_b = gated_delta_chunked(flip_t(qc), flip_t(kc), flip_t(vc), flip_t(bc[:, :, 1]), flip_t(gc[:, :, 1]), s0)
    o_lf, _ = gated_delta_chunked(ql, kl, vl, bl[:, :, 0], gl[:, :, 0], s_f)
    o_lb, _ = gated_delta_chunked(flip_t(ql), flip_t(kl), flip_t(vl), flip_t(bl[:, :, 1]), flip_t(gl[:, :, 1]), s_b)
    o_l = o_lf + flip_t(o_lb)
    o_c = (o_cf + flip_t(o_cb)) if need_ctx else None
    return o_c, o_l


def clamped_swiglu(hgu):
    gate, up = jnp.split(hgu, 2, axis=-1)
    gate = jnp.minimum(gate, SWIGLU_LIMIT)
    up = jnp.clip(up, -SWIGLU_LIMIT, SWIGLU_LIMIT)
    return gate * jax.nn.sigmoid(SWIGLU_ALPHA * gate) * (up + 1.0)


def moe_ffn(h, w_r, b_r, w_up, b_up, w_down, b_down):
    t, d = h.shape
    logits = (h @ w_r + b_r).astype(jnp.float32)
    top_val, top_idx = lax.top_k(logits, TOP_K)
    gates = jax.nn.softmax(top_val, axis=-1)
    m = t * TOP_K
    e_flat = top_idx.reshape(m)
    tok_flat = jnp.arange(m, dtype=jnp.int32) // TOP_K
    order = jnp.argsort(e_flat)
    e_sorted = e_flat[order]
    counts = jnp.bincount(e_flat, length=N_EXPERTS)
    starts = jnp.cumsum(counts) - counts
    padded = (counts + MOE_BLOCK - 1) // MOE_BLOCK * MOE_BLOCK
    ends_p = jnp.cumsum(padded)
    starts_p = ends_p - padded
    dest = starts_p[e_sorted] + jnp.arange(m, dtype=jnp.int32) - starts[e_sorted]
    n_blocks = (m + N_EXPERTS * (MOE_BLOCK - 1) + MOE_BLOCK - 1) // MOE_BLOCK
    p = n_blocks * MOE_BLOCK
    slot_tok = jnp.full((p,), t, jnp.int32).at[dest].set(tok_flat[order])
    slot_gate = jnp.zeros((p,), jnp.float32).at[dest].set(gates.reshape(m)[order])
    block_expert = jnp.minimum(jnp.searchsorted(ends_p, jnp.arange(n_blocks) * MOE_BLOCK, side='right'), N_EXPERTS - 1)
    h_pad = jnp.concatenate([h, jnp.zeros((1, d), h.dtype)], axis=0)

    def expert_block(args):
        tok, e = args
        xb = h_pad[tok]
        hid = clamped_swiglu(xb @ w_up[e] + b_up[e])
        return hid @ w_down[e] + b_down[e]

    ys = lax.map(expert_block, (slot_tok.reshape(n_blocks, MOE_BLOCK), block_expert))
    ys = ys.reshape(p, d) * slot_gate[:, None].astype(h.dtype)
    return jnp.zeros((t + 1, d), h.dtype).at[slot_tok].add(ys)[:t]


def hybrid_mixer(h_c, h_l, need_ctx, rope_a, rope_d, lam_init, w_in, w_out, q_norm, k_norm,
                 v_norm, w_s, b_s, lq1, lk1, lq2, lk2, subln, conv_w, a_log, dt_bias, out_norm):
    b, l, _ = h_l.shape
    lc = h_c.shape[1]
    grp = GQA_HEADS // GQA_KV_HEADS
    (aq_l, ak_l, av_l, bu_l, bv_l, cq_l, ck_l, cv_l, dqkv_l, dz_l, db_l, da_l) = jnp.split(h_l @ w_in, IN_OFFSETS, axis=-1)
    (aq_c, ak_c, av_c, bu_c, bv_c, cq_c, ck_c, cv_c, dqkv_c, dz_c, db_c, da_c) = jnp.split(h_c @ w_in, IN_OFFSETS, axis=-1)

    qa_l = apply_rope(rms_norm(heads(aq_l, GQA_HEADS), q_norm), rope_a)
    ka_l = apply_rope(rms_norm(heads(ak_l, GQA_KV_HEADS), k_norm), rope_a)
    ka_c = rms_norm(heads(ak_c, GQA_KV_HEADS), k_norm)
    va_l, va_c = heads(av_l, GQA_KV_HEADS), heads(av_c, GQA_KV_HEADS)
    out_a_l = gqa_attend(qa_l.reshape(b, l, GQA_KV_HEADS, grp, HEAD_DIM),
                         jnp.concatenate([ka_l, ka_c], axis=1), jnp.concatenate([va_l, va_c], axis=1)).reshape(b, l, GQA_W)

    out_b_l = gmlp_spatial(jax.nn.gelu(bu_l), jax.nn.gelu(bv_l), w_s, b_s, v_norm)

    lam = (jnp.exp(jnp.sum(lq1 * lk1).astype(jnp.float32)) - jnp.exp(jnp.sum(lq2 * lk2).astype(jnp.float32)) + lam_init)
    qd_l, kd_l, kd_c = heads(cq_l, DIFF_HEADS), heads(ck_l, DIFF_HEADS), heads(ck_c, DIFF_HEADS)
    q1_l, q2_l = apply_rope(qd_l[..., :DIFF_DIM], rope_d), apply_rope(qd_l[..., DIFF_DIM:], rope_d)
    k1_all = jnp.concatenate([apply_rope(kd_l[..., :DIFF_DIM], rope_d), kd_c[..., :DIFF_DIM]], axis=1)
    k2_all = jnp.concatenate([apply_rope(kd_l[..., DIFF_DIM:], rope_d), kd_c[..., DIFF_DIM:]], axis=1)
    vd_c = heads(cv_c, DIFF_HEADS)
    vd_all = jnp.concatenate([heads(cv_l, DIFF_HEADS), vd_c], axis=1)
    od_l = diff_attend(q1_l, q2_l, k1_all, k2_all, vd_all, lam)
    out_c_l = (rms_norm(od_l, subln) * (1.0 - lam_init)).reshape(b, l, DIFF_W)

    ctx_in = gdn_inputs(dqkv_c, db_c, da_c, conv_w, a_log, dt_bias)
    lat_in = gdn_inputs(dqkv_l, db_l, da_l, conv_w, a_log, dt_bias)
    o_c, o_l = gdn_bidirectional(ctx_in, lat_in, need_ctx)
    out_d_l = (rms_norm(o_l, out_norm) * jax.nn.silu(heads(dz_l, DN_HEADS))).reshape(b, l, DN_W)

    y_l = jnp.concatenate([out_a_l, out_b_l, out_c_l, out_d_l], axis=-1) @ w_out
    if not need_ctx:
        return None, y_l

    qa_c = rms_norm(heads(aq_c, GQA_HEADS), q_norm).reshape(b, lc, GQA_KV_HEADS, grp, HEAD_DIM)
    out_a_c = gqa_attend(qa_c, ka_c, va_c).reshape(b, lc, GQA_W)
    out_b_c = gmlp_spatial(jax.nn.gelu(bu_c), jax.nn.gelu(bv_c), w_s, b_s, v_norm)
    qd_c = heads(cq_c, DIFF_HEADS)
    od_c = diff_attend(qd_c[..., :DIFF_DIM], qd_c[..., DIFF_DIM:], kd_c[..., :DIFF_DIM], kd_c[..., DIFF_DIM:], vd_c, lam)
    out_c_c = (rms_norm(od_c, subln) * (1.0 - lam_init)).reshape(b, lc, DIFF_W)
    out_d_c = (rms_norm(o_c, out_norm) * jax.nn.silu(heads(dz_c, DN_HEADS))).reshape(b, lc, DN_W)
    y_c = jnp.concatenate([out_a_c, out_b_c, out_c_c, out_d_c], axis=-1) @ w_out
    return y_c, y_l


def setup_inputs(seed: int = 0) -> dict:
    key = jax.random.key(seed)
    ks = jax.random.split(key, 32)
    f32 = jnp.float32

    def nrm(k, shape, scale=1.0):
        return jax.random.normal(k, shape, f32) * scale

    def gain(k, shape):
        return 1.0 + 0.02 * jax.random.normal(k, shape, f32)

    dt = jnp.exp(jax.random.uniform(ks[23], (DEPTH, 2, DN_HEADS), f32, math.log(1e-3), math.log(1e-1)))
    return {
        'x': nrm(ks[0], (BATCH, SEQ, D_MODEL)),
        'c': nrm(ks[1], (BATCH, D_MODEL)),
        'ctx': nrm(ks[2], (BATCH, CTX_LEN, D_MODEL)),
        'c_ctx': nrm(ks[3], (D_MODEL,), 0.5),
        'w_ada': nrm(ks[4], (DEPTH, D_MODEL, 6 * D_MODEL), 0.5 * D_MODEL ** -0.5),
        'b_ada': nrm(ks[5], (DEPTH, 6 * D_MODEL), 0.01),
        'norm_mix': gain(ks[6], (DEPTH, D_MODEL)),
        'norm_ffn': gain(ks[7], (DEPTH, D_MODEL)),
        'w_in': nrm(ks[8], (DEPTH, D_MODEL, D_IN), D_MODEL ** -0.5),
        'w_out': nrm(ks[9], (DEPTH, MIX_WIDTH, D_MODEL), MIX_WIDTH ** -0.5),
        'gqa_q_norm': gain(ks[10], (DEPTH, HEAD_DIM)),
        'gqa_k_norm': gain(ks[11], (DEPTH, HEAD_DIM)),
        'gmlp_v_norm': gain(ks[12], (DEPTH, GMLP_W)),
        'gmlp_w_s': nrm(ks[13], (DEPTH, GMLP_GROUPS, GMLP_CHUNK, GMLP_CHUNK), GMLP_CHUNK ** -0.5),
        'gmlp_b_s': gain(ks[14], (DEPTH, GMLP_GROUPS, GMLP_CHUNK)),
        'diff_lambda_q1': nrm(ks[15], (DEPTH, DIFF_DIM), 0.1),
        'diff_lambda_k1': nrm(ks[16], (DEPTH, DIFF_DIM), 0.1),
        'diff_lambda_q2': nrm(ks[17], (DEPTH, DIFF_DIM), 0.1),
        'diff_lambda_k2': nrm(ks[18], (DEPTH, DIFF_DIM), 0.1),
        'diff_subln': gain(ks[19], (DEPTH, 2 * DIFF_DIM)),
        'dn_conv_w': nrm(ks[20], (DEPTH, DN_CONV, DN_QKV), DN_CONV ** -0.5),
        'dn_a_log': jnp.log(jax.random.uniform(ks[21], (DEPTH, 2, DN_HEADS), f32, 1.0, 16.0)),
        'dn_dt_bias': dt + jnp.log(-jnp.expm1(-dt)),
        'dn_out_norm': gain(ks[22], (DEPTH, DN_DV)),
        'router_w': nrm(ks[24], (DEPTH, D_MODEL, N_EXPERTS), D_MODEL ** -0.5),
        'router_b': nrm(ks[25], (DEPTH, N_EXPERTS), 0.01),
        'exp_w_up': nrm(ks[26], (DEPTH, N_EXPERTS, D_MODEL, 2 * D_EXPERT), D_MODEL ** -0.5),
        'exp_b_up': nrm(ks[27], (DEPTH, N_EXPERTS, 2 * D_EXPERT), 0.01),
        'exp_w_down': nrm(ks[28], (DEPTH, N_EXPERTS, D_EXPERT, D_MODEL), D_EXPERT ** -0.5),
        'exp_b_down': nrm(ks[29], (DEPTH, N_EXPERTS, D_MODEL), 0.01),
        'final_norm': gain(ks[30], (D_MODEL,)),
    }


def reference(x, c, ctx, c_ctx, w_ada, b_ada, norm_mix, norm_ffn, w_in, w_out, gqa_q_norm, gqa_k_norm,
              gmlp_v_norm, gmlp_w_s, gmlp_b_s, diff_lambda_q1, diff_lambda_k1, diff_lambda_q2, diff_lambda_k2,
              diff_subln, dn_conv_w, dn_a_log, dn_dt_bias, dn_out_norm, router_w, router_b,
              exp_w_up, exp_b_up, exp_w_down, exp_b_down, final_norm):
    b, l, d = x.shape
    lc = ctx.shape[1]
    rows = l // GRID_W
    r_idx, c_idx = jnp.meshgrid(jnp.arange(rows), jnp.arange(GRID_W), indexing='ij')
    row_pos = r_idx.reshape(-1).astype(jnp.float32)
    col_pos = c_idx.reshape(-1).astype(jnp.float32)
    rope_a = axial_rope(row_pos, col_pos, HEAD_DIM)
    rope_d = axial_rope(row_pos, col_pos, DIFF_DIM)
    silu_c = jax.nn.silu(c)
    silu_c_ctx = jax.nn.silu(c_ctx)

    for layer in range(DEPTH):
        need_ctx = layer < DEPTH - 1
        lam_init = 0.8 - 0.6 * math.exp(-0.3 * layer)
        mod = jnp.split(silu_c @ w_ada[layer] + b_ada[layer], 6, axis=-1)
        sh1, sc1, g1, sh2, sc2, g2 = (m[:, None, :] for m in mod)
        csh1, csc1, cg1, csh2, csc2, cg2 = jnp.split(silu_c_ctx @ w_ada[layer] + b_ada[layer], 6)

        h_l = modulate(rms_norm(x, norm_mix[layer]), sh1, sc1)
        h_c = modulate(rms_norm(ctx, norm_mix[layer]), csh1, csc1)
        y_c, y_l = hybrid_mixer(h_c, h_l, need_ctx, rope_a, rope_d, lam_init, w_in[layer], w_out[layer],
                                gqa_q_norm[layer], gqa_k_norm[layer], gmlp_v_norm[layer], gmlp_w_s[layer],
                                gmlp_b_s[layer], diff_lambda_q1[layer], diff_lambda_k1[layer],
                                diff_lambda_q2[layer], diff_lambda_k2[layer], diff_subln[layer],
                                dn_conv_w[layer], dn_a_log[layer], dn_dt_bias[layer], dn_out_norm[layer])
        x = x + g1 * y_l
        f_l = modulate(rms_norm(x, norm_ffn[layer]), sh2, sc2)
        if need_ctx:
            ctx = ctx + cg1 * y_c
            f_c = modulate(rms_norm(ctx, norm_ffn[layer]), csh2, csc2)
            tokens = jnp.concatenate([f_l.reshape(b * l, d), f_c.reshape(b * lc, d)], axis=0)
        else:
            tokens = f_l.reshape(b * l, d)
        ffn = moe_ffn(tokens, router_w[layer], router_b[layer], exp_w_up[layer], exp_b_up[layer],
                      exp_w_down[layer], exp_b_down[layer])
        x = x + g2 * ffn[:b * l].reshape(b, l, d)
        if need_ctx:
            ctx = ctx + cg2 * ffn[b * l:].reshape(b, lc, d)

    return rms_norm(x, final_norm)
```

```python
import math
from contextlib import ExitStack
import numpy as np
import concourse.bass as bass
import concourse.mybir as mybir
from concourse.bass_utils import run_bass_kernel_spmd

F32 = mybir.dt.float32
BF16 = mybir.dt.bfloat16
AF = mybir.ActivationFunctionType
ALU = mybir.AluOpType
AX = mybir.AxisListType

COMPUTE = ("pe", "act", "dve", "pool")
NRING = 8
D = 1024
KC = 8
DIN = 2832
OFF = dict(aq=0, ak=256, av=384, bu=512, bv=768, cq=1024, ck=1280, cv=1536, dqkv=1792, dz=2560, db=2816, da=2824)
EPS = 1e-6


class Prog:
    def __init__(self, nc, same_engine_sync=True):
        self.nc = nc
        self.ops = []
        self.last_w = {}
        self.readers = {}
        self.same = same_engine_sync
        self.bdeps = set()
        self.lastop = {}
        self.dmaq = {}

    @staticmethod
    def _isps(t):
        return (isinstance(t, tuple) and t[0] == "ps") or (isinstance(t, str) and t.startswith("ps"))

    def add(self, eng, fn, r=(), w=(), dma=False):
        idx = len(self.ops)
        deps = set(self.bdeps)
        w = list(w) + [t for t in r if self._isps(t)]
        r = [t for t in r if not self._isps(t)]
        for res in r:
            if res in self.last_w:
                deps.add(self.last_w[res])
        for res in w:
            if res in self.last_w:
                deps.add(self.last_w[res])
            for rd in self.readers.get(res, ()):
                deps.add(rd)
        for res in w:
            self.last_w[res] = idx
            self.readers[res] = []
        for res in r:
            self.readers.setdefault(res, []).append(idx)
        self.ops.append(dict(eng=eng, fn=fn, deps=sorted(deps), dma=dma))
        if dma:
            self.dmaq.setdefault(eng, []).append(idx)
        else:
            self.lastop[eng] = idx
        return idx

    def barrier(self):
        b = set(self.lastop.values())
        for q, lst in self.dmaq.items():
            b.update(lst[-NRING:])
        self.bdeps = b
        self.last_w = {}
        self.readers = {}

    def dma(self, q, out, in_, r=(), w=(), **kw):
        return self.add(q, lambda e: e.dma_start(out=out, in_=in_, **kw), r, w, dma=True)

    def tt(self, eng, out, in0, in1, op, r=(), w=()):
        return self.add(eng, lambda e: e.tensor_tensor(out=out, in0=in0, in1=in1, op=op), r, w)

    def ts(self, eng, out, in0, s1, s2=None, op0=ALU.mult, op1=None, r=(), w=(), accum_out=None):
        kw = {}
        if op1 is not None:
            kw["op1"] = op1
        if accum_out is not None:
            kw["accum_out"] = accum_out
        return self.add(eng, lambda e: e.tensor_scalar(out=out, in0=in0, scalar1=s1, scalar2=s2, op0=op0, **kw), r, w)

    def stt(self, eng, out, in0, scalar, in1, op0, op1, r=(), w=()):
        return self.add(eng, lambda e: e.scalar_tensor_tensor(out=out, in0=in0, scalar=scalar, in1=in1, op0=op0, op1=op1), r, w)

    def cp(self, eng, out, in_, r=(), w=()):
        if eng == "act":
            return self.add(eng, lambda e: e.copy(out=out, in_=in_), r, w)
        return self.add(eng, lambda e: e.tensor_copy(out=out, in_=in_), r, w)

    def actv(self, out, in_, func, r=(), w=(), bias=None, scale=1.0, accum_out=None):
        kw = {}
        if bias is not None:
            kw["bias"] = bias
        if accum_out is not None:
            kw["accum_out"] = accum_out
        return self.add("act", lambda e: e.activation(out=out, in_=in_, func=func, scale=scale, **kw), r, w)

    def red(self, eng, out, in_, op, r=(), w=()):
        return self.add(eng, lambda e: e.tensor_reduce(out=out, in_=in_, axis=AX.X, op=op), r, w)

    def recip(self, out, in_, r=(), w=()):
        return self.add("dve", lambda e: e.reciprocal(out=out, in_=in_), r, w)

    def ms(self, eng, ap, val, w=()):
        return self.add(eng, lambda e: e.memset(ap, val), (), w)

    def mm(self, items, r=(), w=()):
        def fn(e):
            ins = None
            for (out, lhsT, rhs, start, stop) in items:
                ins = e.matmul(out, lhsT=lhsT, rhs=rhs, start=start, stop=stop)
            return ins
        return self.add("pe", fn, r, w)

    def tr(self, items, r=(), w=()):
        def fn(e):
            ins = None
            for (out, in_, ident) in items:
                ins = e.transpose(out=out, in_=in_, identity=ident)
            return ins
        return self.add("pe", fn, r, w)

    def emit(self, stack):
        nc = self.nc
        ops = self.ops
        cnt = {}
        for o in ops:
            key = (o["eng"], o["dma"])
            o["pos"] = cnt.get(key, 0)
            cnt[key] = o["pos"] + 1
        seen = {}
        seen_dma = {}
        for o in ops:
            E = o["eng"]
            sd = seen.setdefault(E, {})
            sdd = seen_dma.setdefault(E, set())
            waits = []
            for d in o["deps"]:
                p = ops[d]
                if p["dma"]:
                    if d in sdd:
                        continue
                    sdd.add(d)
                    waits.append(d)
                else:
                    A = p["eng"]
                    if A == E and not o["dma"]:
                        if A == "pe" or not self.same:
                            continue
                    if sd.get(A, -1) >= p["pos"]:
                        continue
                    sd[A] = p["pos"]
                    waits.append(d)
            o["waits"] = waits
            for d in waits:
                ops[d]["sig"] = True
        self.sem = {e: stack.enter_context(nc.semaphore("s_" + e)) for e in COMPUTE}
        queues = sorted({o["eng"] for o in ops if o["dma"]})
        self.ring = {q: [stack.enter_context(nc.semaphore("r_%s_%d" % (q, k))) for k in range(NRING)] for q in queues}
        ccount = {e: 0 for e in COMPUTE}
        ndma = {q: 0 for q in queues}
        for o in ops:
            if o["dma"]:
                k = o["pos"]
                o["sem"] = self.ring[o["eng"]][k % NRING]
                o["val"] = 16 * (k // NRING + 1)
                o["sig"] = True
                ndma[o["eng"]] += 1
            elif o.get("sig"):
                ccount[o["eng"]] += 1
                o["sem"] = self.sem[o["eng"]]
                o["val"] = ccount[o["eng"]]
        per = {}
        for i, o in enumerate(ops):
            per.setdefault(o["eng"], []).append(i)
        self.stats = {e: len(v) for e, v in per.items()}
        self.stats["sig"] = dict(ccount)

        def run(engname, eng):
            for i in per.get(engname, []):
                o = ops[i]
                for d in o["waits"]:
                    p = ops[d]
                    eng.wait_ge(p["sem"], p["val"])
                if o["dma"]:
                    k = o["pos"]
                    if k >= NRING:
                        eng.wait_ge(self.ring[engname][k % NRING], 16 * (k // NRING))
                ins = o["fn"](eng)
                if o.get("sig"):
                    ins.then_inc(o["sem"], 16 if o["dma"] else 1)
            if engname in ndma:
                n = ndma[engname]
                for k in range(min(n, NRING)):
                    last = ((n - 1 - k) // NRING) * NRING + k
                    eng.wait_ge(self.ring[engname][k], 16 * (last // NRING + 1))

        block = stack.enter_context(nc.Block())

        @block.sync
        def _(e):
            run("sp", e)

        @block.scalar
        def _(e):
            run("act", e)

        @block.vector
        def _(e):
            run("dve", e)

        @block.gpsimd
        def _(e):
            run("pool", e)

        @block.tensor
        def _(e):
            run("pe", e)


class Arena:
    def __init__(self, nc, st, words):
        self.t = st.enter_context(nc.sbuf_tensor("arena", [128, words], F32))
        self.off = 0
        self.words = words

    def f32(self, n):
        ap = self.t[:, self.off:self.off + n]
        self.off += n
        assert self.off <= self.words, ("arena overflow", self.off, self.words)
        return ap

    def bf16(self, n):
        wd = (n + 1) // 2
        ap = self.t[:, self.off:self.off + wd].bitcast(BF16)
        self.off += wd
        assert self.off <= self.words, ("arena overflow", self.off, self.words)
        return ap

    def mark(self):
        return self.off

    def reset(self, m):
        self.off = m


def build(L, LC, DEPTH, NE, phases=None, dbg=False):
    T = L + LC
    NT = T // 128
    NTL = L // 128
    NCH = T // 64
    nc = bass.Bass("TRN2", target_bir_lowering=False)

    def din(name, shape, dt=F32):
        return nc.dram_tensor(name, list(shape), dt, kind="ExternalInput").ap()

    I = {}
    I["x0"] = din("x0", [T, D])
    I["scs"] = din("scs", [128, 16])
    I["w_ada"] = din("w_ada", [DEPTH, D, 6 * D])
    I["b_ada"] = din("b_ada", [DEPTH, 128, 48])
    I["nmix"] = din("nmix", [DEPTH, 128, 8])
    I["nffn"] = din("nffn", [DEPTH, 128, 8])
    I["w_in"] = din("w_in", [DEPTH, D, DIN])
    I["w_out"] = din("w_out", [DEPTH, D, D])
    I["gqk"] = din("gqk", [DEPTH, 1, 384])
    I["gvn"] = din("gvn", [DEPTH, 1, 256])
    I["w_s"] = din("w_s", [DEPTH, 4, 128, 128])
    I["b_s"] = din("b_s", [DEPTH, 128, 4])
    I["lam"] = din("lam", [DEPTH, 1, 128])
    I["subln"] = din("subln", [DEPTH, 1, 64])
    I["convw"] = din("convw", [DEPTH, 3, 768])
    I["alog"] = din("alog", [DEPTH, 1, 8])
    I["dtb"] = din("dtb", [DEPTH, 1, 8])
    I["onorm"] = din("onorm", [DEPTH, 1, 64])
    I["w_r"] = din("w_r", [DEPTH, D, NE])
    I["b_r"] = din("b_r", [DEPTH, 1, NE])
    I["w_up"] = din("w_up", [DEPTH, NE, D, 2 * D])
    I["b_up"] = din("b_up", [DEPTH, NE, 128, 16])
    I["w_dn"] = din("w_dn", [DEPTH, NE, D, D])
    I["b_dn"] = din("b_dn", [DEPTH, NE, D])
    I["fnorm"] = din("fnorm", [1, D])
    I["ropa"] = din("ropa", [L, 64])
    I["ropd"] = din("ropd", [L, 32])
    I["ident"] = din("ident", [128, 128])
    I["gmask"] = din("gmask", [2, 64, 5, 64])
    out = nc.dram_tensor("out", [L, D], F32, kind="ExternalOutput").ap()

    X = nc.dram_tensor("X", [T, D], F32).ap()
    PROJ = nc.dram_tensor("PROJ", [T, DIN], F32).ap()
    CAT = nc.dram_tensor("CAT", [T, D], F32).ap()
    MODD = nc.dram_tensor("MODD", [96, 128], F32).ap()
    FTD = nc.dram_tensor("FTD", [128, KC, T], BF16).ap()
    GDNP = nc.dram_tensor("GDNP", [T, 784], F32).ap()
    OF = nc.dram_tensor("OF", [T, 256], F32).ap()
    DBG = {}
    if dbg:
        DBG["PROJ"] = nc.dram_tensor("dPROJ", [T, DIN], F32, kind="ExternalOutput").ap()
        DBG["CAT"] = nc.dram_tensor("dCAT", [T, D], F32, kind="ExternalOutput").ap()
        DBG["X1"] = nc.dram_tensor("dX1", [T, D], F32, kind="ExternalOutput").ap()

    st = ExitStack()
    P = Prog(nc)
    AR = Arena(nc, st, 51 * 1024)
    ps = [st.enter_context(nc.psum_tensor("ps%d" % i, [128, 512], F32)) for i in range(8)]

    def psb(i):
        return ps[i][:, :].bitcast(BF16)

    ident = AR.f32(128)
    identb = AR.bf16(128)
    scs = AR.f32(16)
    ones = AR.f32(128)
    P.dma("sp", ident, I["ident"], w=["ident"])
    P.dma("sp", scs, I["scs"], w=["scs"])
    P.cp("dve", identb, ident, r=["ident"], w=["identb"])
    P.ms("dve", ones, 1.0, w=["ones"])
    sig0 = AR.f32(16)
    P.actv(sig0, scs, AF.Sigmoid, r=["scs"], w=["sig0"])
    P.tt("dve", scs, scs, sig0, ALU.mult, r=["scs", "sig0"], w=["scs"])
    A1 = AR.f32(16)
    B1 = AR.f32(16)
    A2 = AR.f32(16)
    B2 = AR.f32(16)
    GB = AR.f32(4 * D)
    base_mark = AR.mark()

    P.dma("sp", X, I["x0"], w=["X"])
    P.barrier()

    def want(name):
        return phases is None or name in phases

    def phase_mod(l):
        m = AR.mark()
        stg = [AR.f32(KC * 768) for _ in range(2)]
        modf = AR.f32(96)
        bada = AR.f32(48)
        nm = AR.f32(16)
        modt = AR.f32(128)
        P.dma("pool", bada, I["b_ada"][l], w=["bada"])
        P.dma("pool", nm[:, 0:8], I["nmix"][l], w=["nm"])
        P.dma("pool", nm[:, 8:16], I["nffn"][l], w=["nm"])
        scs3 = scs.rearrange("p (k w) -> p k w", w=2)
        for cbk in range(8):
            s3 = stg[cbk % 2].rearrange("p (k n) -> p k n", k=KC)
            for kc in range(KC):
                P.dma("sp" if kc % 2 == 0 else "pool", s3[:, kc, :], I["w_ada"][l, kc * 128:(kc + 1) * 128, cbk * 768:(cbk + 1) * 768], w=[("stg", cbk % 2, kc)])
            for jj in range(6):
                j = cbk * 6 + jj
                P.mm([(ps[0][:, j * 2:j * 2 + 2], s3[:, kc, jj * 128:(jj + 1) * 128], scs3[:, kc, :], kc == 0, kc == KC - 1) for kc in range(KC)],
                     r=[("stg", cbk % 2, kc) for kc in range(KC)] + ["scs"], w=["ps0"])
        mf3 = modf.rearrange("p (j w) -> p j w", w=2)
        P.tt("dve", mf3, ps[0][:, 0:96].rearrange("p (j w) -> p j w", w=2), bada.unsqueeze(2).to_broadcast([128, 48, 2]), ALU.add,
             r=["ps0", "bada"], w=["modf"])
        nm3 = nm.rearrange("p (a k) -> p a k", a=2)
        for (Aap, Bap, jsc, jsh, a) in ((A1, B1, 8, 0, 0), (A2, B2, 32, 24, 1)):
            A3 = Aap.rearrange("p (k w) -> p k w", w=2)
            B3 = Bap.rearrange("p (k w) -> p k w", w=2)
            P.ts("dve", A3, mf3[:, jsc:jsc + 8, :], 1.0, None, op0=ALU.add, r=["modf"], w=[("A", a)])
            P.tt("dve", A3, A3, nm3[:, a, :].unsqueeze(2).to_broadcast([128, 8, 2]), ALU.mult, r=[("A", a), "nm"], w=[("A", a)])
            P.cp("dve", B3, mf3[:, jsh:jsh + 8, :], r=["modf"], w=[("B", a)])
        P.tr([(ps[1][0:96, 0:128], modf, ident)], r=["modf", "ident"], w=["ps1"])
        P.cp("act", modt[0:96, :], ps[1][0:96, 0:128], r=["ps1"], w=["modt"])
        P.dma("sp", MODD, modt[0:96, :], r=["modt"], w=["MODD"])
        md = MODD.rearrange("(j w) p -> w j p", w=2)
        for gi, (j0, wv) in enumerate(((16, 0), (16, 1), (40, 0), (40, 1))):
            src = md[wv, j0:j0 + 8, :]
            P.dma("sp", GB[:, gi * D:(gi + 1) * D].rearrange("p (j q) -> p j q", q=128), src.partition_broadcast(128), r=["MODD"], w=["GB"])
        AR.reset(m)
        P.barrier()

    def norm_tile(xt, sq, col, eng_sq="act"):
        P.actv(sq, xt, AF.Square, r=["xt"], w=["sq", "col"], accum_out=col[:, 0:1])
        P.actv(col[:, 1:2], col[:, 0:1], AF.Sqrt, r=["col"], w=["col1"], bias=EPS, scale=1.0 / D)
        P.recip(col[:, 2:3], col[:, 1:2], r=["col1"], w=["col2"])

    def phase_inproj(l):
        m = AR.mark()
        win = AR.bf16(KC * DIN)
        win3 = win.rearrange("p (k n) -> p k n", k=KC)
        stg = [AR.f32(DIN) for _ in range(2)]
        for kc in range(KC):
            s = stg[kc % 2]
            P.dma("pool", s, I["w_in"][l, kc * 128:(kc + 1) * 128, :], w=[("stg", kc % 2)])
            P.cp("pool", win3[:, kc, :], s, r=[("stg", kc % 2)], w=["win"])
        xt = AR.f32(D)
        sq = AR.f32(D)
        col = AR.f32(4)
        xn = AR.bf16(D)
        hT = AR.bf16(D)
        prj = AR.f32(DIN)
        hT3 = hT.rearrange("p (k t) -> p k t", k=KC)
        for t in range(NT):
            w = 0 if t < NTL else 1
            P.dma("sp", xt, X[t * 128:(t + 1) * 128, :], r=["X"], w=["xt"])
            norm_tile(xt, sq, col)
            P.ts("dve", xn, xt, col[:, 2:3], None, op0=ALU.mult, r=["xt", "col2"], w=["xn"])
            pb = psb(0)
            P.tr([(pb[:, k * 128:(k + 1) * 128], xn[:, k * 128:(k + 1) * 128], identb) for k in range(KC)], r=["xn", "identb"], w=["ps0"])
            A3 = A1.rearrange("p (k w) -> p k w", w=2)
            B3 = B1.rearrange("p (k w) -> p k w", w=2)
            for k in range(KC):
                P.actv(hT3[:, k, :], pb[:, k * 128:(k + 1) * 128], AF.Identity, r=["ps0", ("A", 0), ("B", 0)], w=[("hT", k)],
                       bias=B3[:, k, w:w + 1], scale=A3[:, k, w:w + 1])
            for cb in range(6):
                c0 = cb * 512
                c1 = min(DIN, c0 + 512)
                bank = 1 + (cb % 3)
                P.mm([(ps[bank][:, 0:c1 - c0], hT3[:, k, :], win3[:, k, c0:c1], k == 0, k == KC - 1) for k in range(KC)],
                     r=[("hT", k) for k in range(KC)] + ["win"], w=[("ps", bank)])
                P.cp("dve" if cb % 2 == 0 else "act", prj[:, c0:c1], ps[bank][:, 0:c1 - c0], r=[("ps", bank)], w=[("prj", cb)])
            P.dma("sp", PROJ[t * 128:(t + 1) * 128, :], prj, r=[("prj", cb) for cb in range(6)], w=["PROJ"])
        AR.reset(m)
        P.barrier()
        if dbg and l == 0:
            P.dma("sp", DBG["PROJ"], PROJ, r=["PROJ"], w=["dPROJ"])
            P.barrier()

    def attention(qT, kT, V, nqb, QB, ktiles, vslot, scale, dqk, sink, tag):
        for qb in range(nqb):
            nk = len(ktiles)
            for i, kt in enumerate(ktiles):
                sb = 1 + (i % 2)
                P.mm([(ps[sb][:, 0:QB], kT(kt), qT(qb), True, True)], r=[(tag + "k", kt), (tag + "q", qb)], w=[("ps", sb)])
                pt = PT[i % 2]
                P.actv(pt[:, 0:QB], ps[sb][:, 0:QB], AF.Exp, r=[("ps", sb)], w=[("pt", i % 2)], scale=scale)
                P.mm([(ps[3][0:65, 0:QB], V(kt), pt[:, 0:QB], i == 0, i == nk - 1)], r=[("pt", i % 2), (tag + "v", kt)], w=[("ps", 3)])
            P.cp("dve", OT[0:65, 0:QB], ps[3][0:65, 0:QB], r=[("ps", 3)], w=["ot"])
            for sub in range(QB // 128):
                P.tr([(ps[4][:, 0:65], OT[0:65, sub * 128:(sub + 1) * 128], ident[0:65, 0:65])], r=["ot", "ident"], w=[("ps", 4)])
                sink(qb, sub, ps[4][:, 0:65])

    def phase_attn(l, need_ctx, lam_init):
        nonlocal PT, OT
        m = AR.mark()
        B1buf = AR.bf16(8 * T)
        B2buf = AR.bf16(8 * T)
        VB = AR.bf16(NT * 4 * 65)
        PT = [AR.bf16(512) for _ in range(2)]
        OT = AR.f32(512)
        pin = AR.f32(768)
        tmp = AR.f32(768)
        tmp2 = AR.f32(768)
        qkb = AR.bf16(768)
        col = AR.f32(16)
        gqk = AR.f32(384)
        rop = AR.f32(64)
        ores = AR.f32(64)
        P.dma("pool", gqk, I["gqk"][l, 0].partition_broadcast(128), w=["gqk"])
        QB = min(512, L)
        nqb = L // QB
        VB4 = VB.rearrange("p (t h e) -> p t h e", t=NT, h=4)
        P.ms("pool", VB, 1.0, w=["VB"])
        P.barrier()
        QT = B1buf[0:64, 0:4 * L].rearrange("p (h t) -> p h t", h=4)
        KT = B2buf[0:64, 0:2 * T].rearrange("p (h t) -> p h t", h=2)
        QTC = B1buf[0:64, 4 * L:4 * L + 4 * LC].rearrange("p (h t) -> p h t", h=4)
        for t in range(NT):
            lat = t < NTL
            P.dma("sp", pin[:, 0:512], PROJ[t * 128:(t + 1) * 128, 0:512], w=["pin"])
            if lat:
                P.dma("sp", rop, I["ropa"][t * 128:(t + 1) * 128, :], w=["rop"])
            q3 = pin[:, 0:384].rearrange("p (h d) -> p h d", h=6)
            t3 = tmp[:, 0:384].rearrange("p (h d) -> p h d", h=6)
            u3 = tmp2[:, 0:384].rearrange("p (h d) -> p h d", h=6)
            P.tt("dve", t3, q3, q3, ALU.mult, r=["pin"], w=["tmp"])
            P.red("dve", col[:, 0:6], t3, ALU.add, r=["tmp"], w=["col"])
            P.actv(col[:, 6:12], col[:, 0:6], AF.Sqrt, r=["col"], w=["colb"], bias=EPS, scale=1.0 / 64)
            P.recip(col[:, 0:6], col[:, 6:12], r=["colb"], w=["col"])
            P.tt("dve", t3, q3, col[:, 0:6].unsqueeze(2).to_broadcast([128, 6, 64]), ALU.mult, r=["pin", "col"], w=["tmp"])
            P.tt("dve", tmp[:, 0:384], tmp[:, 0:384], gqk, ALU.mult, r=["tmp", "gqk"], w=["tmp"])
            qb3 = qkb[:, 0:384].rearrange("p (h d) -> p h d", h=6)
            if lat:
                cs = rop[:, 0:32].unsqueeze(1).to_broadcast([128, 6, 32])
                sn = rop[:, 32:64].unsqueeze(1).to_broadcast([128, 6, 32])
                x1 = t3[:, :, 0:32]
                x2 = t3[:, :, 32:64]
                P.tt("dve", u3[:, :, 0:32], x1, cs, ALU.mult, r=["tmp", "rop"], w=["tmp2a"])
                P.tt("pool", u3[:, :, 32:64], x2, sn, ALU.mult, r=["tmp", "rop"], w=["tmp2b"])
                P.tt("dve", qb3[:, :, 0:32], u3[:, :, 0:32], u3[:, :, 32:64], ALU.subtract, r=["tmp2a", "tmp2b"], w=["qkb"])
                P.tt("dve", u3[:, :, 0:32], x1, sn, ALU.mult, r=["tmp", "rop", "qkb"], w=["tmp2a"])
                P.tt("pool", u3[:, :, 32:64], x2, cs, ALU.mult, r=["tmp", "rop", "qkb"], w=["tmp2b"])
                P.tt("dve", qb3[:, :, 32:64], u3[:, :, 0:32], u3[:, :, 32:64], ALU.add, r=["tmp2a", "tmp2b"], w=["qkb"])
            else:
                P.cp("dve", qkb[:, 0:384], tmp[:, 0:384], r=["tmp"], w=["qkb"])
            pb = psb(0)
            P.tr([(pb[0:64, h * 128:(h + 1) * 128], qkb[:, h * 64:(h + 1) * 64], identb) for h in range(6)], r=["qkb", "identb"], w=[("ps", 0)])
            pb3 = pb[0:64, 0:768].rearrange("p (h t) -> p h t", h=6)
            if lat:
                P.cp("act", QT[:, :, t * 128:(t + 1) * 128], pb3[:, 0:4, :], r=[("ps", 0)], w=[("gq", t * 128 // QB)])
            else:
                tc_ = t - NTL
                P.cp("act", QTC[:, :, tc_ * 128:(tc_ + 1) * 128], pb3[:, 0:4, :], r=[("ps", 0)], w=[("gqc", 0)])
            P.cp("act", KT[:, :, t * 128:(t + 1) * 128], pb3[:, 4:6, :], r=[("ps", 0)], w=[("gk", t)])
            P.cp("pool", VB4[:, t, 0:2, 0:64], pin[:, 384:512].rearrange("p (h d) -> p h d", h=2), r=["pin"], w=[("gv", t)])
        P.barrier()
        for h in range(4):
            hk = h // 2

            def sink(qb, sub, res, h=h):
                tok = qb * QB + sub * 128
                P.recip(col[:, 12:13], res[:, 64:65], r=[("ps", 4)], w=["colr"])
                P.ts("dve", ores, res[:, 0:64], col[:, 12:13], None, op0=ALU.mult, r=[("ps", 4), "colr"], w=["ores"])
                P.dma("pool", CAT[tok:tok + 128, h * 64:(h + 1) * 64], ores, r=["ores"], w=[("CATa", h, tok)])
            attention(lambda qb, h=h: QT[:, h, qb * QB:(qb + 1) * QB], lambda kt, hk=hk: KT[:, hk, kt * 128:(kt + 1) * 128],
                      lambda kt, hk=hk: VB4[:, kt, hk, :], nqb, QB, list(range(NT)), None, 0.125, 64, sink, "g")
            if need_ctx:
                def sinkc(qb, sub, res, h=h):
                    tok = L + sub * 128
                    P.recip(col[:, 12:13], res[:, 64:65], r=[("ps", 4)], w=["colr"])
                    P.ts("dve", ores, res[:, 0:64], col[:, 12:13], None, op0=ALU.mult, r=[("ps", 4), "colr"], w=["ores"])
                    P.dma("pool", CAT[tok:tok + 128, h * 64:(h + 1) * 64], ores, r=["ores"], w=[("CATa", h, tok)])
                attention(lambda qb, h=h: QTC[:, h, 0:LC], lambda kt, hk=hk: KT[:, hk, kt * 128:(kt + 1) * 128],
                          lambda kt, hk=hk: VB4[:, kt, hk, :], 1, LC, list(range(NTL, NT)), None, 0.125, 64, sinkc, "gc")
        P.barrier()
        DQ = B1buf[0:32, 0:8 * L].rearrange("p (h t) -> p h t", h=8)
        DK = B2buf[0:32, 0:8 * T].rearrange("p (h t) -> p h t", h=8)
        DQC = B1buf[0:32, 8 * L:8 * L + 8 * LC].rearrange("p (h t) -> p h t", h=8)
        O1 = AR.f32(NT * 64)
        lamt = AR.f32(128)
        subl = AR.f32(64)
        P.dma("pool", lamt, I["lam"][l, 0].partition_broadcast(128), w=["lamt"])
        P.dma("pool", subl, I["subln"][l, 0].partition_broadcast(128), w=["subl"])
        l4 = lamt.rearrange("p (a d) -> p a d", a=4)
        lt = AR.f32(64)
        lt3 = lt.rearrange("p (a d) -> p a d", a=2)
        P.tt("dve", lt3[:, 0, :], l4[:, 0, :], l4[:, 1, :], ALU.mult, r=["lamt"], w=["lt"])
        P.tt("dve", lt3[:, 1, :], l4[:, 2, :], l4[:, 3, :], ALU.mult, r=["lamt", "lt"], w=["lt"])
        lc = AR.f32(8)
        P.red("dve", lc[:, 0:2], lt3, ALU.add, r=["lt"], w=["lc"])
        P.actv(lc[:, 2:4], lc[:, 0:2], AF.Exp, r=["lc"], w=["lc2"])
        P.tt("dve", lc[:, 4:5], lc[:, 3:4], lc[:, 2:3], ALU.subtract, r=["lc2"], w=["lc4"])
        P.ts("dve", lc[:, 5:6], lc[:, 4:5], -float(lam_init), None, op0=ALU.add, r=["lc4"], w=["nlam"])
        for t in range(NT):
            lat = t < NTL
            P.dma("sp", pin, PROJ[t * 128:(t + 1) * 128, 1024:1792], w=["pin"])
            qsrc = pin[:, 0:512]
            if lat:
                P.dma("sp", rop[:, 0:32], I["ropd"][t * 128:(t + 1) * 128, :], w=["rop"])
                s3 = pin[:, 0:512].rearrange("p (h d) -> p h d", h=16)
                u3 = tmp[:, 0:512].rearrange("p (h d) -> p h d", h=16)
                v3 = tmp2[:, 0:512].rearrange("p (h d) -> p h d", h=16)
                qb3 = qkb[:, 0:512].rearrange("p (h d) -> p h d", h=16)
                cs = rop[:, 0:16].unsqueeze(1).to_broadcast([128, 16, 16])
                sn = rop[:, 16:32].unsqueeze(1).to_broadcast([128, 16, 16])
                x1 = s3[:, :, 0:16]
                x2 = s3[:, :, 16:32]
                P.tt("dve", u3[:, :, 0:16], x1, cs, ALU.mult, r=["pin", "rop"], w=["tmpa"])
                P.tt("pool", u3[:, :, 16:32], x2, sn, ALU.mult, r=["pin", "rop"], w=["tmpb"])
                P.tt("dve", qb3[:, :, 0:16], u3[:, :, 0:16], u3[:, :, 16:32], ALU.subtract, r=["tmpa", "tmpb"], w=["qkb"])
                P.tt("dve", v3[:, :, 0:16], x1, sn, ALU.mult, r=["pin", "rop"], w=["tmp2a"])
                P.tt("pool", v3[:, :, 16:32], x2, cs, ALU.mult, r=["pin", "rop"], w=["tmp2b"])
                P.tt("dve", qb3[:, :, 16:32], v3[:, :, 0:16], v3[:, :, 16:32], ALU.add, r=["tmp2a", "tmp2b"], w=["qkb"])
            else:
                P.cp("dve", qkb[:, 0:512], pin[:, 0:512], r=["pin"], w=["qkb"])
            for half in range(2):
                bank = half
                pb = psb(bank)
                P.tr([(pb[0:32, mI * 128:(mI + 1) * 128], qkb[:, half * 256 + mI * 32:half * 256 + (mI + 1) * 32], identb) for mI in range(8)],
                     r=["qkb", "identb"], w=[("ps", bank)])
                pb3 = pb[0:32, 0:1024].rearrange("p (h t) -> p h t", h=8)
                if half == 0:
                    if lat:
                        P.cp("act", DQ[:, :, t * 128:(t + 1) * 128], pb3, r=[("ps", bank)], w=[("dq", t * 128 // QB)])
                    else:
                        tc_ = t - NTL
                        P.cp("act", DQC[:, :, tc_ * 128:(tc_ + 1) * 128], pb3, r=[("ps", bank)], w=[("dqc", 0)])
                else:
                    P.cp("act", DK[:, :, t * 128:(t + 1) * 128], pb3, r=[("ps", bank)], w=[("dk", t)])
            P.cp("pool", VB4[:, t, :, 0:64], pin[:, 512:768].rearrange("p (h d) -> p h d", h=4), r=["pin"], w=[("dv", t)])
        O13 = O1.rearrange("p (t d) -> p t d", d=64)
        dscale = 32 ** -0.5
        P.barrier()

        def finish(res_tile, tok, h):
            P.tt("dve", tmp2[:, 0:64], tmp[:, 0:64], tmp[:, 0:64], ALU.mult, r=["od"], w=["od2"])
            P.red("dve", col[:, 0:1], tmp2[:, 0:64], ALU.add, r=["od2"], w=["c0"])
            P.actv(col[:, 1:2], col[:, 0:1], AF.Sqrt, r=["c0"], w=["c1"], bias=EPS, scale=1.0 / 64)
            P.recip(col[:, 2:3], col[:, 1:2], r=["c1"], w=["c2"])
            P.stt("dve", ores, tmp[:, 0:64], col[:, 2:3], subl, ALU.mult, ALU.mult, r=["od", "c2", "subl"], w=["ores"])
            P.ts("dve", ores, ores, 1.0 - float(lam_init), None, op0=ALU.mult, r=["ores"], w=["ores"])
            P.dma("pool", CAT[tok:tok + 128, 512 + h * 64:512 + (h + 1) * 64], ores, r=["ores"], w=[("CATc", h, tok)])

        for h in range(4):
            for mI in range(2):
                hm = 2 * h + mI

                def sink(qb, sub, res, h=h, mI=mI, base=0, QBv=QB):
                    tok = base + qb * QBv + sub * 128
                    tI = tok // 128
                    P.recip(col[:, 12:13], res[:, 64:65], r=[("ps", 4)], w=["colr"])
                    if mI == 0:
                        P.ts("dve", O13[:, tI, :], res[:, 0:64], col[:, 12:13], None, op0=ALU.mult, r=[("ps", 4), "colr"], w=[("o1", tI)])
                    else:
                        P.ts("dve", tmp2[:, 64:128], res[:, 0:64], col[:, 12:13], None, op0=ALU.mult, r=[("ps", 4), "colr"], w=["o2"])
                        P.stt("dve", tmp[:, 0:64], tmp2[:, 64:128], lc[:, 5:6], O13[:, tI, :], ALU.mult, ALU.add, r=["o2", "nlam", ("o1", tI)], w=["od"])
                        finish(None, tok, h)
                attention(lambda qb, hm=hm: DQ[:, hm, qb * QB:(qb + 1) * QB], lambda kt, hm=hm: DK[:, hm, kt * 128:(kt + 1) * 128],
                          lambda kt, h=h: VB4[:, kt, h, :], nqb, QB, list(range(NT)), None, dscale, 32, sink, "d")
                if need_ctx:
                    attention(lambda qb, hm=hm: DQC[:, hm, 0:LC], lambda kt, hm=hm: DK[:, hm, kt * 128:(kt + 1) * 128],
                              lambda kt, h=h: VB4[:, kt, h, :], 1, LC, list(range(NTL, NT)), None, dscale, 32,
                              lambda qb, sub, res, s=sink: s(qb, sub, res, base=L, QBv=LC), "dc")
        AR.reset(m)
        P.barrier()

    PT = None
    OT = None

    def gelu_tanh(dst, src, t1, t2, n, tagr):
        P.tt("dve", t1, src, src, ALU.mult, r=tagr, w=["g_t1"])
        P.ts("dve", t1, t1, 0.044715, 1.0, op0=ALU.mult, op1=ALU.add, r=["g_t1"], w=["g_t1"])
        P.tt("dve", t1, t1, src, ALU.mult, r=["g_t1"] + tagr, w=["g_t1"])
        P.actv(t2, t1, AF.Sigmoid, r=["g_t1"], w=["g_t2"], scale=1.5957691216057308)
        P.tt("dve", dst, t2, src, ALU.mult, r=["g_t2"] + tagr, w=["g_dst"])

    def phase_gmlp(l, need_ctx):
        m = AR.mark()
        wsT = AR.f32(512)
        wst = AR.f32(128)
        bs = AR.f32(4)
        gv = AR.f32(256)
        pin = AR.f32(512)
        t1 = AR.f32(512)
        t2 = AR.f32(512)
        ge = AR.f32(512)
        vn = AR.f32(256)
        ob = AR.f32(256)
        col = AR.f32(4)
        P.dma("pool", bs, I["b_s"][l], w=["bs"])
        P.dma("pool", gv, I["gvn"][l, 0].partition_broadcast(128), w=["gv"])
        for g in range(4):
            P.dma("sp", wst, I["w_s"][l, g], w=["wst"])
            P.tr([(ps[0][:, 0:128], wst, ident)], r=["wst", "ident"], w=["ps0"])
            P.cp("dve", wsT[:, g * 128:(g + 1) * 128], ps[0][:, 0:128], r=["ps0"], w=["wsT"])
        ntiles = NT if need_ctx else NTL
        for t in range(ntiles):
            P.dma("sp", pin, PROJ[t * 128:(t + 1) * 128, 512:1024], w=["pin"])
            gelu_tanh(ge, pin, t1, t2, 512, ["pin"])
            P.tt("dve", t1[:, 0:256], ge[:, 256:512], ge[:, 256:512], ALU.mult, r=["g_dst"], w=["g_t1"])
            P.red("dve", col[:, 0:1], t1[:, 0:256], ALU.add, r=["g_t1"], w=["c0"])
            P.actv(col[:, 1:2], col[:, 0:1], AF.Sqrt, r=["c0"], w=["c1"], bias=EPS, scale=1.0 / 256)
            P.recip(col[:, 2:3], col[:, 1:2], r=["c1"], w=["c2"])
            P.stt("dve", vn, ge[:, 256:512], col[:, 2:3], gv, ALU.mult, ALU.mult, r=["g_dst", "c2", "gv"], w=["vn"])
            P.mm([(ps[1][:, g * 64:(g + 1) * 64], wsT[:, g * 128:(g + 1) * 128], vn[:, g * 64:(g + 1) * 64], True, True) for g in range(4)],
                 r=["vn", "wsT"], w=["ps1"])
            for g in range(4):
                P.stt("dve", ob[:, g * 64:(g + 1) * 64], ps[1][:, g * 64:(g + 1) * 64], bs[:, g:g + 1], ge[:, g * 64:(g + 1) * 64], ALU.add, ALU.mult,
                      r=["ps1", "bs", "g_dst"], w=["ob"])
            P.dma("pool", CAT[t * 128:(t + 1) * 128, 256:512], ob, r=["ob"], w=[("CATb", t)])
        AR.reset(m)
        P.barrier()

    def phase_gdn(l, need_ctx):
        m = AR.mark()
        cw = AR.f32(3 * 768)
        alg = AR.f32(16)
        xp = AR.f32(768)
        xc = AR.f32(768)
        xnx = AR.f32(768)
        y = AR.f32(768)
        y2 = AR.f32(768)
        gp = AR.f32(784)
        ba = AR.f32(16)
        col = AR.f32(16)
        for k in range(3):
            P.dma("pool", cw[:, k * 768:(k + 1) * 768], I["convw"][l, k].partition_broadcast(128), w=["cw"])
        P.dma("pool", alg[:, 0:8], I["alog"][l, 0].partition_broadcast(128), w=["alg"])
        P.dma("pool", alg[:, 8:16], I["dtb"][l, 0].partition_broadcast(128), w=["alg"])
        P.actv(alg[:, 0:8], alg[:, 0:8], AF.Exp, r=["alg"], w=["alg"])
        c0 = OFF["dqkv"]
        for t in range(NT):
            seq0 = 0 if t < NTL else L
            seq1 = L if t < NTL else T
            r0 = t * 128
            P.dma("sp", xc, PROJ[r0:r0 + 128, c0:c0 + 768], w=["xc"])
            P.dma("sp", ba, PROJ[r0:r0 + 128, OFF["db"]:OFF["db"] + 16], w=["ba"])
            if r0 == seq0:
                P.ms("pool", xp[0:1, :], 0.0, w=["xp"])
                P.dma("sp", xp[1:128, :], PROJ[r0:r0 + 127, c0:c0 + 768], w=["xp"])
            else:
                P.dma("sp", xp, PROJ[r0 - 1:r0 + 127, c0:c0 + 768], w=["xp"])
            if r0 + 128 == seq1:
                P.ms("pool", xnx, 0.0, w=["xnx"])
                P.dma("sp", xnx[0:127, :], PROJ[r0 + 1:r0 + 128, c0:c0 + 768], w=["xnx"])
            else:
                P.dma("sp", xnx, PROJ[r0 + 1:r0 + 129, c0:c0 + 768], w=["xnx"])
            P.tt("dve", y, xp, cw[:, 0:768], ALU.mult, r=["xp", "cw"], w=["y"])
            P.tt("pool", y2, xc, cw[:, 768:1536], ALU.mult, r=["xc", "cw"], w=["y2"])
            P.tt("dve", y, y, y2, ALU.add, r=["y", "y2"], w=["y"])
            P.tt("pool", y2, xnx, cw[:, 1536:2304], ALU.mult, r=["xnx", "cw", "y"], w=["y2"])
            P.tt("dve", y, y, y2, ALU.add, r=["y", "y2"], w=["y"])
            P.actv(y2, y, AF.Sigmoid, r=["y"], w=["y2"])
            P.tt("dve", y, y, y2, ALU.mult, r=["y", "y2"], w=["y"])
            y3 = y[:, 0:512].rearrange("p (h d) -> p h d", h=8)
            s3 = y2[:, 0:512].rearrange("p (h d) -> p h d", h=8)
            P.tt("dve", s3, y3, y3, ALU.mult, r=["y"], w=["y2"])
            P.red("dve", col[:, 0:8], s3, ALU.add, r=["y2"], w=["c0"])
            P.actv(col[:, 8:16], col[:, 0:8], AF.Sqrt, r=["c0"], w=["c1"], bias=EPS, scale=1.0)
            P.recip(col[:, 0:8], col[:, 8:16], r=["c1"], w=["c2"])
            P.ts("dve", col[:, 0:4], col[:, 0:4], 0.125, None, op0=ALU.mult, r=["c2"], w=["c2"])
            g3 = gp[:, 0:512].rearrange("p (h d) -> p h d", h=8)
            P.tt("dve", g3, y3, col[:, 0:8].unsqueeze(2).to_broadcast([128, 8, 64]), ALU.mult, r=["y", "c2"], w=["gp"])
            P.cp("pool", gp[:, 512:768], y[:, 512:768], r=["y"], w=["gpv"])
            P.actv(gp[:, 768:776], ba[:, 0:8], AF.Sigmoid, r=["ba"], w=["gpb"])
            P.tt("dve", ba[:, 8:16], ba[:, 8:16], alg[:, 8:16], ALU.add, r=["ba", "alg"], w=["ba"])
            P.actv(ba[:, 8:16], ba[:, 8:16], AF.Exp, r=["ba"], w=["ba"])
            P.actv(ba[:, 8:16], ba[:, 8:16], AF.Ln, r=["ba"], w=["ba"], bias=1.0)
            P.stt("dve", gp[:, 776:784], ba[:, 8:16], -1.0, alg[:, 0:8], ALU.mult, ALU.mult, r=["ba", "alg"], w=["gpg"])
            P.dma("sp", GDNP[r0:r0 + 128, :], gp, r=["gp", "gpv", "gpb", "gpg"], w=[("GDNP", t)])
        AR.reset(m)
        P.barrier()
        if phases is not None and "nosweep" in phases:
            return
        msk = AR.f32(5 * 64)
        ones64 = ones[0:64, 0:64]
        ST = AR.f32(256)
        ST3 = ST[0:64, :].rearrange("p (h d) -> p h d", h=4)
        cin = AR.f32(784)
        KTs = AR.f32(256)
        QTs = AR.f32(256)
        KBT = AR.f32(256)
        kb = AR.f32(256)
        cg = AR.f32(16)
        Rm = AR.f32(256)
        E = AR.f32(256)
        X1 = AR.f32(256)
        X2 = AR.f32(256)
        tm = AR.f32(256)
        Mb = [AR.f32(256) for _ in range(2)]
        MTb = [AR.f32(256) for _ in range(2)]
        QKT = AR.f32(256)
        yv = AR.f32(512)
        WT = AR.f32(256)
        QTT = AR.f32(256)
        qt_ = AR.f32(256)
        kd = AR.f32(256)
        nv = AR.f32(256)
        osb = AR.f32(256)
        egb = AR.f32(256)
        ofl = AR.f32(256)
        zt = AR.f32(256)
        on = AR.f32(64)
        col2 = AR.f32(16)
        P.dma("pool", on, I["onorm"][l, 0].partition_broadcast(128), w=["on"])
        order_f = list(range(L // 64, NCH)) + list(range(0, L // 64))
        order_b = list(range(NCH - 1, L // 64 - 1, -1)) + list(range(L // 64 - 1, -1, -1))
        for dr in range(2):
            P.dma("sp", msk[0:64, :], I["gmask"][dr].rearrange("p a f -> p (a f)"), w=["msk"])
            U = msk[0:64, 0:64]
            nMS = msk[0:64, 64:128].unsqueeze(1).to_broadcast([64, 4, 64])
            nMST = msk[0:64, 128:192].unsqueeze(1).to_broadcast([64, 4, 64])
            MIT = msk[0:64, 192:256].unsqueeze(1).to_broadcast([64, 4, 64])
            P.ms("dve", ST, 0.0, w=["ST"])
            last = 63 if dr == 0 else 0
            for n in (order_f if dr == 0 else order_b):
                r0 = n * 64
                lat = r0 < L
                c = cin[0:64, :]
                P.dma("sp", c, GDNP[r0:r0 + 64, :], r=[("GDNP", r0 // 128)], w=["cin"])
                q3 = c[:, 0:256].rearrange("p (h d) -> p h d", h=4)
                k3 = c[:, 256:512].rearrange("p (h d) -> p h d", h=4)
                v3 = c[:, 512:768].rearrange("p (h d) -> p h d", h=4)
                beta = c[:, 768 + 4 * dr:772 + 4 * dr]
                gg = c[:, 776 + 4 * dr:780 + 4 * dr]
                kb3 = kb[0:64, :].rearrange("p (h d) -> p h d", h=4)
                P.tt("dve", kb3, k3, beta.unsqueeze(2).to_broadcast([64, 4, 64]), ALU.mult, r=["cin"], w=["kb"])
                P.tr([(ps[0][0:64, h * 64:(h + 1) * 64], c[:, 256 + h * 64:256 + (h + 1) * 64], ident[0:64, 0:64]) for h in range(4)] +
                     [(ps[0][0:64, 256 + h * 64:256 + (h + 1) * 64], c[:, h * 64:(h + 1) * 64], ident[0:64, 0:64]) for h in range(4)],
                     r=["cin", "ident"], w=[("ps", 0)])
                P.cp("act", KTs[0:64, :], ps[0][0:64, 0:256], r=[("ps", 0)], w=["KTs"])
                P.cp("act", QTs[0:64, :], ps[0][0:64, 256:512], r=[("ps", 0)], w=["QTs"])
                P.tr([(ps[1][0:64, h * 64:(h + 1) * 64], kb[0:64, h * 64:(h + 1) * 64], ident[0:64, 0:64]) for h in range(4)], r=["kb", "ident"], w=[("ps", 1)])
                P.mm([(ps[1][0:64, 256:260], U, gg, True, True)], r=["msk", "cin"], w=[("ps", 1)])
                P.cp("act", KBT[0:64, :], ps[1][0:64, 0:256], r=[("ps", 1)], w=["KBT"])
                P.cp("dve", cg[0:64, 0:4], ps[1][0:64, 256:260], r=[("ps", 1)], w=["cgc"])
                R3 = Rm[0:64, :].rearrange("p (h d) -> p h d", h=4)
                P.tt("dve", R3, gg.unsqueeze(2).to_broadcast([64, 4, 64]), U.unsqueeze(1).to_broadcast([64, 4, 64]), ALU.mult, r=["cin", "msk"], w=["Rm"])
                P.mm([(ps[2][0:64, 0:256], ones64, Rm[0:64, :], True, True)], r=["Rm", "ones"], w=[("ps", 2)])
                B3 = ps[2][0:64, 0:256].rearrange("p (h d) -> p h d", h=4)
                E3 = E[0:64, :].rearrange("p (h d) -> p h d", h=4)
                P.tt("dve", E3, B3, cg[0:64, 0:4].unsqueeze(2).to_broadcast([64, 4, 64]), ALU.subtract, r=[("ps", 2), "cgc"], w=["E"])
                P.tt("dve", cg[0:64, 8:12], B3[:, :, last], cg[0:64, 0:4], ALU.subtract, r=[("ps", 2), "cgc"], w=["cg8"])
                P.actv(cg[0:64, 8:12], cg[0:64, 8:12], AF.Exp, r=["cg8"], w=["cg8"])
                P.actv(cg[0:64, 12:16], B3[:, :, last], AF.Exp, r=[("ps", 2)], w=["cg12"])
                P.actv(cg[0:64, 4:8], cg[0:64, 0:4], AF.Exp, r=["cgc"], w=["cg4"])
                P.ts("dve", X1[0:64, :], E[0:64, :], 0.0, None, op0=ALU.min, r=["E"], w=["X1"])
                P.ts("dve", X2[0:64, :], E[0:64, :], -1.0, 0.0, op0=ALU.mult, op1=ALU.min, r=["E"], w=["X2"])
                P.actv(X1[0:64, :], X1[0:64, :], AF.Exp, r=["X1"], w=["X1"])
                P.actv(X2[0:64, :], X2[0:64, :], AF.Exp, r=["X2"], w=["X2"])
                P.mm([(ps[3][0:64, h * 64:(h + 1) * 64], KBT[0:64, h * 64:(h + 1) * 64], KTs[0:64, h * 64:(h + 1) * 64], True, True) for h in range(4)] +
                     [(ps[3][0:64, 256 + h * 64:256 + (h + 1) * 64], KTs[0:64, h * 64:(h + 1) * 64], KBT[0:64, h * 64:(h + 1) * 64], True, True) for h in range(4)],
                     r=["KBT", "KTs"], w=[("ps", 3)])
                P.mm([(ps[4][0:64, h * 64:(h + 1) * 64], KTs[0:64, h * 64:(h + 1) * 64], QTs[0:64, h * 64:(h + 1) * 64], True, True) for h in range(4)],
                     r=["QTs", "KTs"], w=[("ps", 4)])
                M0 = Mb[0][0:64, :]
                MT0 = MTb[0][0:64, :]
                tm3 = tm[0:64, :].rearrange("p (h d) -> p h d", h=4)
                P.tt("dve", tm3, X2[0:64, :].rearrange("p (h d) -> p h d", h=4), nMS, ALU.mult, r=["X2", "msk"], w=["tm"])
                P.tt("dve", M0, ps[3][0:64, 0:256], tm[0:64, :], ALU.mult, r=[("ps", 3), "tm"], w=[("M", 0)])
                P.tt("dve", tm3, X1[0:64, :].rearrange("p (h d) -> p h d", h=4), nMST, ALU.mult, r=["X1", "msk", ("M", 0)], w=["tm"])
                P.tt("dve", MT0, ps[3][0:64, 256:512], tm[0:64, :], ALU.mult, r=[("ps", 3), "tm"], w=[("MT", 0)])
                P.tt("dve", tm3, X1[0:64, :].rearrange("p (h d) -> p h d", h=4), MIT, ALU.mult, r=["X1", "msk", ("MT", 0)], w=["tm"])
                P.tt("dve", QKT[0:64, :], ps[4][0:64, 0:256], tm[0:64, :], ALU.mult, r=[("ps", 4), "tm"], w=["QKT"])
                y4 = yv[0:64, :].rearrange("p (h e) -> p h e", h=4)
                P.tt("dve", y4[:, :, 0:64], v3, beta.unsqueeze(2).to_broadcast([64, 4, 64]), ALU.mult, r=["cin"], w=["yv"])
                P.tt("dve", y4[:, :, 64:128], kb3, cg[0:64, 4:8].unsqueeze(2).to_broadcast([64, 4, 64]), ALU.mult, r=["kb", "cg4"], w=["yv"])
                qt3 = qt_[0:64, :].rearrange("p (h d) -> p h d", h=4)
                kd3 = kd[0:64, :].rearrange("p (h d) -> p h d", h=4)
                P.tt("pool", qt3, q3, cg[0:64, 4:8].unsqueeze(2).to_broadcast([64, 4, 64]), ALU.mult, r=["cin", "cg4"], w=["qt"])
                P.tt("pool", kd3, k3, cg[0:64, 8:12].unsqueeze(2).to_broadcast([64, 4, 64]), ALU.mult, r=["cin", "cg8"], w=["kd"])
                P.tt("pool", egb[0:64, :].rearrange("p (h d) -> p h d", h=4), ones[0:64, 0:1].unsqueeze(2).to_broadcast([64, 4, 64]),
                     cg[0:64, 12:16].unsqueeze(2).to_broadcast([64, 4, 64]), ALU.mult, r=["ones", "cg12"], w=["egb"])
                P.tr([(ps[4][0:64, 256 + h * 64:256 + (h + 1) * 64], qt_[0:64, h * 64:(h + 1) * 64], ident[0:64, 0:64]) for h in range(4)],
                     r=["qt", "ident"], w=[("ps", 4)])
                P.cp("act", QTT[0:64, :], ps[4][0:64, 256:512], r=[("ps", 4)], w=["QTT"])
                for lv in range(6):
                    Mc = Mb[lv % 2][0:64, :]
                    MTc = MTb[lv % 2][0:64, :]
                    P.mm([(ps[5][0:64, h * 128:(h + 1) * 128], MTc[:, h * 64:(h + 1) * 64], yv[0:64, h * 128:(h + 1) * 128], True, True) for h in range(4)],
                         r=[("MT", lv % 2), "yv"], w=[("ps", 5)])
                    if lv < 5:
                        P.mm([(ps[6][0:64, h * 64:(h + 1) * 64], MTc[:, h * 64:(h + 1) * 64], Mc[:, h * 64:(h + 1) * 64], True, True) for h in range(4)] +
                             [(ps[6][0:64, 256 + h * 64:256 + (h + 1) * 64], Mc[:, h * 64:(h + 1) * 64], MTc[:, h * 64:(h + 1) * 64], True, True) for h in range(4)],
                             r=[("MT", lv % 2), ("M", lv % 2)], w=[("ps", 6)])
                    P.tt("dve", yv[0:64, :], yv[0:64, :], ps[5][0:64, :], ALU.add, r=["yv", ("ps", 5)], w=["yv"])
                    if lv < 5:
                        P.cp("act", Mb[(lv + 1) % 2][0:64, :], ps[6][0:64, 0:256], r=[("ps", 6)], w=[("M", (lv + 1) % 2)])
                        P.cp("act", MTb[(lv + 1) % 2][0:64, :], ps[6][0:64, 256:512], r=[("ps", 6)], w=[("MT", (lv + 1) % 2)])
                P.tr([(ps[6][0:64, h * 64:(h + 1) * 64], yv[0:64, h * 128 + 64:h * 128 + 128], ident[0:64, 0:64]) for h in range(4)], r=["yv", "ident"], w=[("ps", 6)])
                P.cp("act", WT[0:64, :], ps[6][0:64, 0:256], r=[("ps", 6)], w=["WT"])
                P.mm([(ps[7][0:64, h * 64:(h + 1) * 64], WT[0:64, h * 64:(h + 1) * 64], ST[0:64, h * 64:(h + 1) * 64], True, True) for h in range(4)],
                     r=["WT", "ST"], w=[("ps", 7)])
                nv3 = nv[0:64, :].rearrange("p (h d) -> p h d", h=4)
                P.tt("dve", nv3, y4[:, :, 0:64], ps[7][0:64, 0:256].rearrange("p (h d) -> p h d", h=4), ALU.subtract, r=["yv", ("ps", 7)], w=["nv"])
                items = []
                for h in range(4):
                    items.append((ps[7][0:64, 256 + h * 64:256 + (h + 1) * 64], QTT[0:64, h * 64:(h + 1) * 64], ST[0:64, h * 64:(h + 1) * 64], True, False))
                    items.append((ps[7][0:64, 256 + h * 64:256 + (h + 1) * 64], QKT[0:64, h * 64:(h + 1) * 64], nv[0:64, h * 64:(h + 1) * 64], False, True))
                P.mm(items, r=["QTT", "ST", "QKT", "nv"], w=[("ps", 7)])
                P.mm([(ps[2][0:64, 256 + h * 64:256 + (h + 1) * 64], kd[0:64, h * 64:(h + 1) * 64], nv[0:64, h * 64:(h + 1) * 64], True, True) for h in range(4)],
                     r=["kd", "nv"], w=[("ps", 2)])
                P.tt("dve", ST[0:64, :], ST[0:64, :], egb[0:64, :], ALU.mult, r=["ST", "egb"], w=["ST"])
                P.tt("dve", ST[0:64, :], ST[0:64, :], ps[2][0:64, 256:512], ALU.add, r=["ST", ("ps", 2)], w=["ST"])
                need_o = lat or need_ctx
                if need_o:
                    if dr == 0:
                        P.cp("act", osb[0:64, :], ps[7][0:64, 256:512], r=[("ps", 7)], w=["osb"])
                        P.dma("sp", OF[r0:r0 + 64, :], osb[0:64, :], r=["osb"], w=[("OF", n)])
                    else:
                        P.dma("sp", ofl[0:64, :], OF[r0:r0 + 64, :], r=[("OF", n)], w=["ofl"])
                        P.dma("sp", zt[0:64, :], PROJ[r0:r0 + 64, OFF["dz"]:OFF["dz"] + 256], w=["zt"])
                        P.tt("dve", osb[0:64, :], ofl[0:64, :], ps[7][0:64, 256:512], ALU.add, r=["ofl", ("ps", 7)], w=["osb"])
                        o3 = osb[0:64, :].rearrange("p (h d) -> p h d", h=4)
                        s3 = ofl[0:64, :].rearrange("p (h d) -> p h d", h=4)
                        P.tt("dve", s3, o3, o3, ALU.mult, r=["osb"], w=["ofl"])
                        P.red("dve", col2[0:64, 0:4], s3, ALU.add, r=["ofl"], w=["c0"])
                        P.actv(col2[0:64, 4:8], col2[0:64, 0:4], AF.Sqrt, r=["c0"], w=["c1"], bias=EPS, scale=1.0 / 64)
                        P.recip(col2[0:64, 0:4], col2[0:64, 4:8], r=["c1"], w=["c2"])
                        P.tt("dve", o3, o3, col2[0:64, 0:4].unsqueeze(2).to_broadcast([64, 4, 64]), ALU.mult, r=["osb", "c2"], w=["osb"])
                        P.tt("dve", o3, o3, on[0:64, :].unsqueeze(1).to_broadcast([64, 4, 64]), ALU.mult, r=["osb", "on"], w=["osb"])
                        P.actv(ofl[0:64, :], zt[0:64, :], AF.Sigmoid, r=["zt", "ofl"], w=["ofl"])
                        P.tt("dve", zt[0:64, :], zt[0:64, :], ofl[0:64, :], ALU.mult, r=["zt", "ofl"], w=["zt"])
                        P.tt("dve", osb[0:64, :], osb[0:64, :], zt[0:64, :], ALU.mult, r=["osb", "zt"], w=["osb"])
                        P.dma("sp", CAT[r0:r0 + 64, 768:1024], osb[0:64, :], r=["osb"], w=[("CATd", n)])
            P.barrier()
        AR.reset(m)
        P.barrier()

    def phase_outproj(l, need_ctx):
        m = AR.mark()
        wo = AR.bf16(KC * D)
        wo3 = wo.rearrange("p (k n) -> p k n", k=KC)
        stg = [AR.f32(D) for _ in range(2)]
        for kc in range(KC):
            s = stg[kc % 2]
            P.dma("pool", s, I["w_out"][l, kc * 128:(kc + 1) * 128, :], w=[("stg", kc % 2)])
            P.cp("pool", wo3[:, kc, :], s, r=[("stg", kc % 2)], w=["wo"])
        ct = AR.f32(D)
        cb = AR.bf16(D)
        cT = AR.bf16(D)
        xt = AR.f32(D)
        yy = AR.f32(D)
        cT3 = cT.rearrange("p (k t) -> p k t", k=KC)
        ntiles = NT if need_ctx else NTL
        for t in range(ntiles):
            w = 0 if t < NTL else 1
            P.dma("sp", ct, CAT[t * 128:(t + 1) * 128, :], w=["ct"])
            P.dma("sp", xt, X[t * 128:(t + 1) * 128, :], r=[("X", t)], w=["xt"])
            P.cp("dve", cb, ct, r=["ct"], w=["cb"])
            pb = psb(0)
            P.tr([(pb[:, k * 128:(k + 1) * 128], cb[:, k * 128:(k + 1) * 128], identb) for k in range(KC)], r=["cb", "identb"], w=[("ps", 0)])
            P.cp("act", cT, pb[:, 0:1024], r=[("ps", 0)], w=["cT"])
            for hf in range(2):
                P.mm([(ps[1 + hf][:, 0:512], cT3[:, k, :], wo3[:, k, hf * 512:(hf + 1) * 512], k == 0, k == KC - 1) for k in range(KC)],
                     r=["cT", "wo"], w=[("ps", 1 + hf)])
                P.tt("dve", yy[:, hf * 512:(hf + 1) * 512], ps[1 + hf][:, 0:512], GB[:, w * D + hf * 512:w * D + (hf + 1) * 512], ALU.mult,
                     r=[("ps", 1 + hf), "GB"], w=[("yy", hf)])
                P.tt("pool", xt[:, hf * 512:(hf + 1) * 512], xt[:, hf * 512:(hf + 1) * 512], yy[:, hf * 512:(hf + 1) * 512], ALU.add,
                     r=["xt", ("yy", hf)], w=["xt"])
            P.dma("sp", X[t * 128:(t + 1) * 128, :], xt, r=["xt"], w=[("X", t)])
        AR.reset(m)
        P.barrier()
        if dbg and l == 0:
            P.dma("sp", DBG["CAT"], CAT, w=["dCAT"])
            P.dma("sp", DBG["X1"], X, w=["dX1"])
            P.barrier()

    def phase_ffn(l, need_ctx, final):
        m = AR.mark()
        ntiles = NT if need_ctx else NTL
        gates = AR.f32(NT * NE)
        gates3 = gates.rearrange("p (t e) -> p t e", e=NE)
        wr = AR.f32(KC * NE)
        wr3 = wr.rearrange("p (k e) -> p k e", k=KC)
        rb = AR.f32(NE)
        P.dma("pool", wr3, I["w_r"][l].rearrange("(k p) e -> p k e", p=128), w=["wr"])
        P.dma("pool", rb, I["b_r"][l, 0].partition_broadcast(128), w=["rb"])
        m2 = AR.mark()
        xt = AR.f32(D)
        sq = AR.f32(D)
        xn = AR.f32(D)
        fT = AR.f32(D)
        fTb = AR.bf16(D)
        col = AR.f32(16)
        lg = AR.f32(NE)
        ex = AR.f32(NE)
        top8 = AR.f32(8)
        fT3 = fT.rearrange("p (k t) -> p k t", k=KC)
        A3 = A2.rearrange("p (k w) -> p k w", w=2)
        B3 = B2.rearrange("p (k w) -> p k w", w=2)
        for t in range(ntiles):
            w = 0 if t < NTL else 1
            P.dma("sp", xt, X[t * 128:(t + 1) * 128, :], r=[("X", t)], w=["xt"])
            norm_tile(xt, sq, col)
            P.ts("dve", xn, xt, col[:, 2:3], None, op0=ALU.mult, r=["xt", "col2"], w=["xn"])
            for hf in range(2):
                P.tr([(ps[hf][:, k * 128:(k + 1) * 128], xn[:, (hf * 4 + k) * 128:(hf * 4 + k + 1) * 128], ident) for k in range(4)],
                     r=["xn", "ident"], w=[("ps", hf)])
                for k in range(4):
                    kk = hf * 4 + k
                    P.actv(fT3[:, kk, :], ps[hf][:, k * 128:(k + 1) * 128], AF.Identity, r=[("ps", hf), ("A", 1), ("B", 1)], w=[("fT", kk)],
                           bias=B3[:, kk, w:w + 1], scale=A3[:, kk, w:w + 1])
            P.cp("pool", fTb, fT, r=[("fT", k) for k in range(KC)], w=["fTb"])
            P.dma("pool", FTD[:, :, t * 128:(t + 1) * 128], fTb.rearrange("p (k t) -> p k t", k=KC), r=["fTb"], w=[("FTD", t)])
            P.mm([(ps[2][:, 0:NE], fT3[:, k, :], wr3[:, k, :], k == 0, k == KC - 1) for k in range(KC)], r=[("fT", k) for k in range(KC)] + ["wr"], w=[("ps", 2)])
            P.tt("dve", lg, ps[2][:, 0:NE], rb, ALU.add, r=[("ps", 2), "rb"], w=["lg"])
            P.add("dve", lambda e: e.max(out=top8, in_=lg), r=["lg"], w=["top8"])
            P.ts("dve", col[:, 4:5], top8[:, 0:1], -1.0, None, op0=ALU.mult, r=["top8"], w=["nmax"])
            P.actv(ex, lg, AF.Exp, r=["lg", "nmax"], w=["ex"], bias=col[:, 4:5])
            P.ts("dve", lg, lg, top8[:, TOPK - 1:TOPK], None, op0=ALU.is_ge, r=["lg", "top8"], w=["lg"])
            P.tt("dve", ex, ex, lg, ALU.mult, r=["ex", "lg"], w=["ex"])
            P.red("dve", col[:, 5:6], ex, ALU.add, r=["ex"], w=["gs"])
            P.recip(col[:, 6:7], col[:, 5:6], r=["gs"], w=["gr"])
            P.ts("dve", gates3[:, t, :], ex, col[:, 6:7], None, op0=ALU.mult, r=["ex", "gr"], w=[("gates", t)])
        AR.reset(m2)
        P.barrier()
        bdn = AR.f32(D)
        gT = AR.f32(128)
        bup = AR.f32(16)
        TG = 8
        groups = [list(range(g0, min(ntiles, g0 + TG))) for g0 in range(0, ntiles, TG)]
        fg = AR.bf16(KC * TG * 128)
        acc = AR.f32(TG * D)
        acc3 = acc.rearrange("p (t d) -> p t d", d=D)
        wu = [AR.bf16(KC * 2 * D) for _ in range(2)]
        wd = [AR.bf16(KC * D) for _ in range(1)]
        su = [AR.f32(D) for _ in range(3)]
        hid = [AR.bf16(KC * 512) for _ in range(2)]
        Gc = AR.f32(512)
        Sg = AR.f32(512)
        Uc = AR.f32(512)
        xt = AR.f32(D)
        sq = AR.f32(D)
        col = AR.f32(8)
        fn_ = AR.f32(D)
        if final:
            P.dma("pool", fn_, I["fnorm"][0].partition_broadcast(128), w=["fn"])
        P.dma("pool", bdn[0:NE, :], I["b_dn"][l], w=["bdn"])
        wcount = 0
        for gi, grp in enumerate(groups):
            ntg = len(grp)
            ntok = ntg * 128
            t0 = grp[0]
            fg3 = fg[:, 0:KC * ntok].rearrange("p (k t) -> p k t", k=KC)
            P.dma("sp", fg3, FTD[:, :, t0 * 128:t0 * 128 + ntok], r=[("FTD", t) for t in grp], w=["fg"])
            for ti, t in enumerate(grp):
                P.tr([(ps[7][0:NE, 0:128], gates3[:, t, :], ident)], r=[("gates", t), "ident"], w=[("ps", 7)])
                P.cp("act", gT[0:NE, :], ps[7][0:NE, 0:128], r=[("ps", 7)], w=["gT"])
                for hf in range(2):
                    P.mm([(ps[5 + hf][:, 0:512], gT[0:NE, :], bdn[0:NE, hf * 512:(hf + 1) * 512], True, True)], r=["gT", "bdn"], w=[("ps", 5 + hf)])
                    P.cp("dve", acc3[:, ti, hf * 512:(hf + 1) * 512], ps[5 + hf][:, 0:512], r=[("ps", 5 + hf)], w=[("acc", ti, hf)])
            blocks = [(b0, min(512, ntok - b0)) for b0 in range(0, ntok, 512)]
            for e in range(NE):
                par = wcount % 2
                wcount += 1
                wu3 = wu[par].rearrange("p (k n) -> p k n", k=KC)
                wd3 = wd[0].rearrange("p (k n) -> p k n", k=KC)
                sc_ = 0
                for kc in range(KC):
                    for hf in range(2):
                        s = su[sc_ % 3]
                        P.dma("sp", s, I["w_up"][l, e, kc * 128:(kc + 1) * 128, hf * D:(hf + 1) * D], w=[("su", sc_ % 3)])
                        P.cp("pool" if sc_ % 2 == 0 else "act", wu3[:, kc, hf * D:(hf + 1) * D], s, r=[("su", sc_ % 3)], w=[("wu", par, kc)])
                        sc_ += 1
                for kc in range(KC):
                    s = su[sc_ % 3]
                    P.dma("sp", s, I["w_dn"][l, e, kc * 128:(kc + 1) * 128, :], w=[("su", sc_ % 3)])
                    P.cp("pool" if sc_ % 2 == 0 else "act", wd3[:, kc, :], s, r=[("su", sc_ % 3)], w=[("wd", 0, kc)])
                    sc_ += 1
                P.dma("pool", bup, I["b_up"][l, e], w=["bup"])
                for bi, (b0, bn) in enumerate(blocks):
                    hb = hid[bi % 2]
                    hb3 = hb.rearrange("p (j t) -> p j t", j=KC)
                    for j in range(KC):
                        pg = (2 * j) % 4
                        pu = (2 * j + 1) % 4
                        P.mm([(ps[pg][:, 0:bn], wu3[:, k, j * 128:(j + 1) * 128], fg3[:, k, b0:b0 + bn], k == 0, k == KC - 1) for k in range(KC)],
                             r=[("wu", par, k) for k in range(KC)] + ["fg"], w=[("ps", pg)])
                        P.mm([(ps[pu][:, 0:bn], wu3[:, k, D + j * 128:D + (j + 1) * 128], fg3[:, k, b0:b0 + bn], k == 0, k == KC - 1) for k in range(KC)],
                             r=[("wu", par, k) for k in range(KC)] + ["fg"], w=[("ps", pu)])
                        P.ts("dve", Gc[:, 0:bn], ps[pg][:, 0:bn], bup[:, j:j + 1], SWL, op0=ALU.add, op1=ALU.min, r=[("ps", pg), "bup"], w=["Gc"])
                        P.actv(Sg[:, 0:bn], Gc[:, 0:bn], AF.Sigmoid, r=["Gc"], w=["Sg"], scale=SWA)
                        P.ts("dve", Uc[:, 0:bn], ps[pu][:, 0:bn], bup[:, 8 + j:9 + j], SWL, op0=ALU.add, op1=ALU.min, r=[("ps", pu), "bup"], w=["Uc"])
                        P.ts("pool", Uc[:, 0:bn], Uc[:, 0:bn], -SWL, 1.0, op0=ALU.max, op1=ALU.add, r=["Uc"], w=["Uc"])
                        P.tt("pool", Gc[:, 0:bn], Gc[:, 0:bn], Sg[:, 0:bn], ALU.mult, r=["Gc", "Sg"], w=["Gc"])
                        P.tt("pool", hb3[:, j, 0:bn], Gc[:, 0:bn], Uc[:, 0:bn], ALU.mult, r=["Gc", "Uc"], w=[("hid", bi % 2, j)])
                    for sub in range(bn // 128):
                        ti = (b0 + sub * 128) // 128
                        t = grp[ti]
                        for hf in range(2):
                            P.mm([(ps[4 + 2 * (sub % 2) + hf][:, 0:512], hb3[:, j, sub * 128:(sub + 1) * 128], wd3[:, j, hf * 512:(hf + 1) * 512], j == 0, j == KC - 1) for j in range(KC)],
                                 r=[("hid", bi % 2, j) for j in range(KC)] + [("wd", 0, k) for k in range(KC)], w=[("ps", 4 + 2 * (sub % 2) + hf)])
                            P.stt("dve", acc3[:, ti, hf * 512:(hf + 1) * 512], ps[4 + 2 * (sub % 2) + hf][:, 0:512], gates3[:, t, e:e + 1], acc3[:, ti, hf * 512:(hf + 1) * 512],
                                  ALU.mult, ALU.add, r=[("ps", 4 + 2 * (sub % 2) + hf), ("acc", ti, hf), ("gates", t)], w=[("acc", ti, hf)])
            for ti, t in enumerate(grp):
                w = 0 if t < NTL else 1
                P.dma("sp", xt, X[t * 128:(t + 1) * 128, :], r=[("X", t)], w=["xt"])
                P.tt("dve", acc3[:, ti, :], acc3[:, ti, :], GB[:, (2 + w) * D:(3 + w) * D], ALU.mult, r=[("acc", ti, 0), ("acc", ti, 1), "GB"], w=[("acc", ti, 0), ("acc", ti, 1)])
                P.tt("dve", xt, xt, acc3[:, ti, :], ALU.add, r=["xt", ("acc", ti, 0), ("acc", ti, 1)], w=["xt"])
                if final:
                    norm_tile(xt, sq, col)
                    P.stt("dve", xt, xt, col[:, 2:3], fn_, ALU.mult, ALU.mult, r=["xt", "col2", "fn"], w=["xt"])
                    P.dma("sp", out[t * 128:(t + 1) * 128, :], xt, r=["xt"], w=[("out", t)])
                else:
                    P.dma("sp", X[t * 128:(t + 1) * 128, :], xt, r=["xt"], w=[("X", t)])
        AR.reset(m)
        P.barrier()

    TOPK = 4
    SWL = 7.0
    SWA = 1.702
    for l in range(DEPTH):
        need_ctx = l < DEPTH - 1
        lam_init = 0.8 - 0.6 * math.exp(-0.3 * l)
        phase_mod(l)
        phase_inproj(l)
        if want("attn"):
            phase_attn(l, need_ctx, lam_init)
        if want("gmlp"):
            phase_gmlp(l, need_ctx)
        if want("gdn"):
            phase_gdn(l, need_ctx)
        phase_outproj(l, need_ctx)
        if want("ffn"):
            phase_ffn(l, need_ctx, l == DEPTH - 1)
    P.emit(st)
    st.close()
    return nc, P


def _consts(L):
    rows = L // 64
    r_idx, c_idx = np.meshgrid(np.arange(rows), np.arange(64), indexing="ij")
    rp = r_idx.reshape(-1).astype(np.float32)
    cp = c_idx.reshape(-1).astype(np.float32)

    def rope(dim):
        n = dim // 4
        inv = np.power(np.float32(10000.0), -np.arange(n, dtype=np.float32) / n).astype(np.float32)
        ang = np.concatenate([rp[:, None] * inv, cp[:, None] * inv], axis=-1).astype(np.float32)
        return np.concatenate([np.cos(ang), np.sin(ang)], axis=-1).astype(np.float32)

    idx = np.arange(64)
    gm = np.zeros((2, 64, 5, 64), np.float32)
    for dr in range(2):
        if dr == 0:
            U = (idx[:, None] <= idx[None, :])
            mS = (idx[:, None] > idx[None, :])
            mI = (idx[:, None] >= idx[None, :])
        else:
            U = (idx[:, None] >= idx[None, :])
            mS = (idx[:, None] < idx[None, :])
            mI = (idx[:, None] <= idx[None, :])
        gm[dr, :, 0, :] = U
        gm[dr, :, 1, :] = -mS.astype(np.float32)
        gm[dr, :, 2, :] = -mS.T.astype(np.float32)
        gm[dr, :, 3, :] = mI.T.astype(np.float32)
    return rope(64), rope(32), gm


def make_in_maps(inp, L, LC, DEPTH, NE, ncores):
    f = lambda a: np.ascontiguousarray(np.asarray(a, dtype=np.float32))
    ropa, ropd, gm = _consts(L)
    shared = {}
    shared["w_ada"] = f(inp["w_ada"])
    shared["b_ada"] = f(np.asarray(inp["b_ada"]).reshape(DEPTH, 48, 128).transpose(0, 2, 1))
    shared["nmix"] = f(np.asarray(inp["norm_mix"]).reshape(DEPTH, 8, 128).transpose(0, 2, 1))
    shared["nffn"] = f(np.asarray(inp["norm_ffn"]).reshape(DEPTH, 8, 128).transpose(0, 2, 1))
    shared["w_in"] = f(inp["w_in"])
    shared["w_out"] = f(inp["w_out"])
    gq = np.asarray(inp["gqa_q_norm"])
    gk = np.asarray(inp["gqa_k_norm"])
    shared["gqk"] = f(np.concatenate([np.tile(gq, (1, 4)), np.tile(gk, (1, 2))], axis=1).reshape(DEPTH, 1, 384))
    shared["gvn"] = f(np.asarray(inp["gmlp_v_norm"]).reshape(DEPTH, 1, 256))
    shared["w_s"] = f(inp["gmlp_w_s"])
    shared["b_s"] = f(np.asarray(inp["gmlp_b_s"]).transpose(0, 2, 1))
    shared["lam"] = f(np.concatenate([np.asarray(inp["diff_lambda_q1"]), np.asarray(inp["diff_lambda_k1"]),
                                      np.asarray(inp["diff_lambda_q2"]), np.asarray(inp["diff_lambda_k2"])], axis=1).reshape(DEPTH, 1, 128))
    shared["subln"] = f(np.asarray(inp["diff_subln"]).reshape(DEPTH, 1, 64))
    shared["convw"] = f(inp["dn_conv_w"])
    shared["alog"] = f(np.asarray(inp["dn_a_log"]).reshape(DEPTH, 1, 8))
    shared["dtb"] = f(np.asarray(inp["dn_dt_bias"]).reshape(DEPTH, 1, 8))
    shared["onorm"] = f(np.asarray(inp["dn_out_norm"]).reshape(DEPTH, 1, 64))
    shared["w_r"] = f(inp["router_w"])
    shared["b_r"] = f(np.asarray(inp["router_b"]).reshape(DEPTH, 1, NE))
    shared["w_up"] = f(inp["exp_w_up"])
    shared["b_up"] = f(np.asarray(inp["exp_b_up"]).reshape(DEPTH, NE, 16, 128).transpose(0, 1, 3, 2))
    shared["w_dn"] = f(inp["exp_w_down"])
    shared["b_dn"] = f(inp["exp_b_down"])
    shared["fnorm"] = f(np.asarray(inp["final_norm"]).reshape(1, D))
    shared["ropa"] = ropa
    shared["ropd"] = ropd
    shared["ident"] = np.eye(128, dtype=np.float32)
    shared["gmask"] = gm
    x = np.asarray(inp["x"])
    ctx = np.asarray(inp["ctx"])
    c = np.asarray(inp["c"])
    cc = np.asarray(inp["c_ctx"])
    maps = []
    for b in range(ncores):
        mp = dict(shared)
        mp["x0"] = f(np.concatenate([x[b], ctx[b]], axis=0))
        s = np.stack([c[b].reshape(8, 128).T, cc.reshape(8, 128).T], axis=-1)
        mp["scs"] = f(s.reshape(128, 16))
        maps.append(mp)
    return maps


_CACHE = {}


def kernel(**inputs):
    B, L, _ = inputs["x"].shape
    LC = inputs["ctx"].shape[1]
    DEPTH = inputs["w_in"].shape[0]
    NE = inputs["router_w"].shape[2]
    key = (L, LC, DEPTH, NE)
    if key not in _CACHE:
        _CACHE[key] = build(L, LC, DEPTH, NE)[0]
    nc = _CACHE[key]
    maps = make_in_maps(inputs, L, LC, DEPTH, NE, B)
    res = run_bass_kernel_spmd(nc, maps, core_ids=list(range(B)))
    return np.stack([res.results[b]["out"] for b in range(B)], axis=0).astype(np.float32)
```

```python
import math
from contextlib import ExitStack
import numpy as np
import concourse.bass as bass
import concourse.mybir as mybir
from concourse.bass_utils import run_bass_kernel_spmd

F32 = mybir.dt.float32
BF16 = mybir.dt.bfloat16
AF = mybir.ActivationFunctionType
ALU = mybir.AluOpType
AX = mybir.AxisListType

COMPUTE = ("pe", "act", "dve", "pool")
NRING = 8
EMBED_WAITS = True
D = 1024
KC = 8
DIN = 2832
OFF = dict(aq=0, ak=256, av=384, bu=512, bv=768, cq=1024, ck=1280, cv=1536, dqkv=1792, dz=2560, db=2816, da=2824)
EPS = 1e-6


class Prog:
    def __init__(self, nc, same_engine_sync=True):
        self.nc = nc
        self.ops = []
        self.last_w = {}
        self.readers = {}
        self.same = same_engine_sync
        self.bdeps = set()
        self.lastop = {}
        self.dmaq = {}

    @staticmethod
    def _isps(t):
        return (isinstance(t, tuple) and t[0] == "ps") or (isinstance(t, str) and t.startswith("ps"))

    def add(self, eng, fn, r=(), w=(), dma=False):
        idx = len(self.ops)
        deps = set(self.bdeps)
        w = list(w) + [t for t in r if self._isps(t)]
        r = [t for t in r if not self._isps(t)]
        for res in r:
            if res in self.last_w:
                deps.add(self.last_w[res])
        for res in w:
            if res in self.last_w:
                deps.add(self.last_w[res])
            for rd in self.readers.get(res, ()):
                deps.add(rd)
        for res in w:
            self.last_w[res] = idx
            self.readers[res] = []
        for res in r:
            self.readers.setdefault(res, []).append(idx)
        self.ops.append(dict(eng=eng, fn=fn, deps=sorted(deps), dma=dma))
        if dma:
            self.dmaq.setdefault(eng, []).append(idx)
        else:
            self.lastop[eng] = idx
        return idx

    def barrier(self):
        b = set(self.lastop.values())
        for q, lst in self.dmaq.items():
            b.update(lst[-NRING:])
        self.bdeps = b
        self.last_w = {}
        self.readers = {}

    def dma(self, q, out, in_, r=(), w=(), **kw):
        return self.add(q, lambda e: e.dma_start(out=out, in_=in_, **kw), r, w, dma=True)

    def tt(self, eng, out, in0, in1, op, r=(), w=()):
        return self.add(eng, lambda e: e.tensor_tensor(out=out, in0=in0, in1=in1, op=op), r, w)

    def ts(self, eng, out, in0, s1, s2=None, op0=ALU.mult, op1=None, r=(), w=(), accum_out=None):
        kw = {}
        if op1 is not None:
            kw["op1"] = op1
        if accum_out is not None:
            kw["accum_out"] = accum_out
        return self.add(eng, lambda e: e.tensor_scalar(out=out, in0=in0, scalar1=s1, scalar2=s2, op0=op0, **kw), r, w)

    def stt(self, eng, out, in0, scalar, in1, op0, op1, r=(), w=()):
        return self.add(eng, lambda e: e.scalar_tensor_tensor(out=out, in0=in0, scalar=scalar, in1=in1, op0=op0, op1=op1), r, w)

    def cp(self, eng, out, in_, r=(), w=()):
        if eng == "act":
            return self.add(eng, lambda e: e.copy(out=out, in_=in_), r, w)
        return self.add(eng, lambda e: e.tensor_copy(out=out, in_=in_), r, w)

    def actv(self, out, in_, func, r=(), w=(), bias=None, scale=1.0, accum_out=None):
        kw = {}
        if bias is not None:
            kw["bias"] = bias
        if accum_out is not None:
            kw["accum_out"] = accum_out
        return self.add("act", lambda e: e.activation(out=out, in_=in_, func=func, scale=scale, **kw), r, w)

    def red(self, eng, out, in_, op, r=(), w=()):
        return self.add(eng, lambda e: e.tensor_reduce(out=out, in_=in_, axis=AX.X, op=op), r, w)

    def recip(self, out, in_, r=(), w=()):
        return self.add("dve", lambda e: e.reciprocal(out=out, in_=in_), r, w)

    def ms(self, eng, ap, val, w=()):
        return self.add(eng, lambda e: e.memset(ap, val), (), w)

    def mm(self, items, r=(), w=()):
        def fn(e):
            ins = None
            first = None
            for (out, lhsT, rhs, start, stop) in items:
                ins = e.matmul(out, lhsT=lhsT, rhs=rhs, start=start, stop=stop)
                if first is None:
                    first = ins
            return (first, ins)
        return self.add("pe", fn, r, w)

    def tr(self, items, r=(), w=()):
        def fn(e):
            ins = None
            first = None
            for (out, in_, ident) in items:
                ins = e.transpose(out=out, in_=in_, identity=ident)
                if first is None:
                    first = ins
            return (first, ins)
        return self.add("pe", fn, r, w)

    def emit(self, stack):
        nc = self.nc
        ops = self.ops
        cnt = {}
        for o in ops:
            key = (o["eng"], o["dma"])
            o["pos"] = cnt.get(key, 0)
            cnt[key] = o["pos"] + 1
        clocks = {}
        seen_dma = {}
        for o in ops:
            E = o["eng"]
            clock = clocks.setdefault(E, {})
            sdd = seen_dma.setdefault(E, set())
            waits = []
            for d in sorted(o["deps"], reverse=True):
                p = ops[d]
                if p["dma"]:
                    if d in sdd:
                        continue
                    sdd.add(d)
                    waits.append(d)
                    for a, v in p["vcs"].items():
                        if clock.get(a, -1) < v:
                            clock[a] = v
                else:
                    A = p["eng"]
                    if A == E and not o["dma"]:
                        if A == "pe" or not self.same:
                            continue
                    if clock.get(A, -1) >= p["pos"]:
                        continue
                    waits.append(d)
                    for a, v in p["vcd"].items():
                        if clock.get(a, -1) < v:
                            clock[a] = v
            o["waits"] = waits
            o["vcs"] = dict(clock)
            if not o["dma"]:
                vd = dict(clock)
                if vd.get(E, -1) < o["pos"]:
                    vd[E] = o["pos"]
                o["vcd"] = vd
            for d in waits:
                ops[d]["sig"] = True
        self.sem = {e: stack.enter_context(nc.semaphore("s_" + e)) for e in COMPUTE}
        queues = sorted({o["eng"] for o in ops if o["dma"]})
        self.ring = {q: [stack.enter_context(nc.semaphore("r_%s_%d" % (q, k))) for k in range(NRING)] for q in queues}
        ccount = {e: 0 for e in COMPUTE}
        ndma = {q: 0 for q in queues}
        for o in ops:
            if o["dma"]:
                k = o["pos"]
                o["sem"] = self.ring[o["eng"]][k % NRING]
                o["val"] = 16 * (k // NRING + 1)
                o["sig"] = True
                ndma[o["eng"]] += 1
            elif o.get("sig"):
                ccount[o["eng"]] += 1
                o["sem"] = self.sem[o["eng"]]
                o["val"] = ccount[o["eng"]]
        per = {}
        for i, o in enumerate(ops):
            per.setdefault(o["eng"], []).append(i)
        self.stats = {e: len(v) for e, v in per.items()}
        self.stats["sig"] = dict(ccount)

        def run(engname, eng):
            for i in per.get(engname, []):
                o = ops[i]
                waits = list(o["waits"])
                emb = None
                if EMBED_WAITS and waits and not o["dma"]:
                    emb = waits.pop()
                for d in waits:
                    p = ops[d]
                    eng.wait_ge(p["sem"], p["val"])
                if o["dma"]:
                    k = o["pos"]
                    if k >= NRING:
                        eng.wait_ge(self.ring[engname][k % NRING], 16 * (k // NRING))
                ins = o["fn"](eng)
                first, last = ins if isinstance(ins, tuple) else (ins, ins)
                if emb is not None:
                    p = ops[emb]
                    first._wait_ge(p["sem"], p["val"])
                if o.get("sig"):
                    last.then_inc(o["sem"], 16 if o["dma"] else 1)
            if engname in ndma:
                n = ndma[engname]
                for k in range(min(n, NRING)):
                    last = ((n - 1 - k) // NRING) * NRING + k
                    eng.wait_ge(self.ring[engname][k], 16 * (last // NRING + 1))

        block = stack.enter_context(nc.Block())

        @block.sync
        def _(e):
            run("sp", e)

        @block.scalar
        def _(e):
            run("act", e)

        @block.vector
        def _(e):
            run("dve", e)

        @block.gpsimd
        def _(e):
            run("pool", e)

        @block.tensor
        def _(e):
            run("pe", e)


class Arena:
    def __init__(self, nc, st, words):
        self.t = st.enter_context(nc.sbuf_tensor("arena", [128, words], F32))
        self.off = 0
        self.words = words

    def f32(self, n):
        ap = self.t[:, self.off:self.off + n]
        self.off += n
        assert self.off <= self.words, ("arena overflow", self.off, self.words)
        return ap

    def bf16(self, n):
        wd = (n + 1) // 2
        ap = self.t[:, self.off:self.off + wd].bitcast(BF16)
        self.off += wd
        assert self.off <= self.words, ("arena overflow", self.off, self.words)
        return ap

    def mark(self):
        return self.off

    def reset(self, m):
        self.off = m


def build(L, LC, DEPTH, NE, phases=None, dbg=False):
    T = L + LC
    NT = T // 128
    NTL = L // 128
    NCH = T // 64
    nc = bass.Bass("TRN2", target_bir_lowering=False)

    def din(name, shape, dt=F32):
        return nc.dram_tensor(name, list(shape), dt, kind="ExternalInput").ap()

    I = {}
    I["x0"] = din("x0", [T, D])
    I["scs"] = din("scs", [128, 16])
    I["w_ada"] = din("w_ada", [DEPTH, D, 6 * D])
    I["b_ada"] = din("b_ada", [DEPTH, 128, 48])
    I["nmix"] = din("nmix", [DEPTH, 128, 8])
    I["nffn"] = din("nffn", [DEPTH, 128, 8])
    I["w_in"] = din("w_in", [DEPTH, D, DIN])
    I["w_out"] = din("w_out", [DEPTH, D, D])
    I["gqk"] = din("gqk", [DEPTH, 1, 384])
    I["gvn"] = din("gvn", [DEPTH, 1, 256])
    I["w_s"] = din("w_s", [DEPTH, 4, 128, 128])
    I["b_s"] = din("b_s", [DEPTH, 128, 4])
    I["lam"] = din("lam", [DEPTH, 1, 128])
    I["subln"] = din("subln", [DEPTH, 1, 64])
    I["convw"] = din("convw", [DEPTH, 3, 768])
    I["alog"] = din("alog", [DEPTH, 1, 8])
    I["dtb"] = din("dtb", [DEPTH, 1, 8])
    I["onorm"] = din("onorm", [DEPTH, 1, 64])
    I["w_r"] = din("w_r", [DEPTH, D, NE])
    I["b_r"] = din("b_r", [DEPTH, 1, NE])
    I["w_up"] = din("w_up", [DEPTH, NE, D, 2 * D])
    I["b_up"] = din("b_up", [DEPTH, NE, 128, 16])
    I["w_dn"] = din("w_dn", [DEPTH, NE, D, D])
    I["b_dn"] = din("b_dn", [DEPTH, NE, D])
    I["fnorm"] = din("fnorm", [1, D])
    I["ropa"] = din("ropa", [L, 64])
    I["ropd"] = din("ropd", [L, 32])
    I["ident"] = din("ident", [128, 128])
    I["gmask"] = din("gmask", [2, 64, 5, 64])
    out = nc.dram_tensor("out", [L, D], F32, kind="ExternalOutput").ap()

    X = nc.dram_tensor("X", [T, D], F32).ap()
    PROJ = nc.dram_tensor("PROJ", [T, DIN], F32).ap()
    CAT = nc.dram_tensor("CAT", [T, D], F32).ap()
    MODD = nc.dram_tensor("MODD", [96, 128], F32).ap()
    FTD = nc.dram_tensor("FTD", [128, KC, T], BF16).ap()
    GDNP = nc.dram_tensor("GDNP", [T, 784], F32).ap()
    OF = nc.dram_tensor("OF", [T, 256], F32).ap()
    DBG = {}
    if dbg:
        DBG["PROJ"] = nc.dram_tensor("dPROJ", [T, DIN], F32, kind="ExternalOutput").ap()
        DBG["CAT"] = nc.dram_tensor("dCAT", [T, D], F32, kind="ExternalOutput").ap()
        DBG["X1"] = nc.dram_tensor("dX1", [T, D], F32, kind="ExternalOutput").ap()

    st = ExitStack()
    P = Prog(nc)
    AR = Arena(nc, st, 51 * 1024)
    ps = [st.enter_context(nc.psum_tensor("ps%d" % i, [128, 512], F32)) for i in range(8)]

    def psb(i):
        return ps[i][:, :].bitcast(BF16)

    ident = AR.f32(128)
    identb = AR.bf16(128)
    scs = AR.f32(16)
    ones = AR.f32(128)
    P.dma("sp", ident, I["ident"], w=["ident"])
    P.dma("sp", scs, I["scs"], w=["scs"])
    P.cp("dve", identb, ident, r=["ident"], w=["identb"])
    P.ms("dve", ones, 1.0, w=["ones"])
    sig0 = AR.f32(16)
    P.actv(sig0, scs, AF.Sigmoid, r=["scs"], w=["sig0"])
    P.tt("dve", scs, scs, sig0, ALU.mult, r=["scs", "sig0"], w=["scs"])
    A1 = AR.f32(16)
    B1 = AR.f32(16)
    A2 = AR.f32(16)
    B2 = AR.f32(16)
    GB = AR.f32(4 * D)
    base_mark = AR.mark()

    P.dma("sp", X, I["x0"], w=["X"])
    P.barrier()

    def want(name):
        return phases is None or name in phases

    def phase_mod(l):
        m = AR.mark()
        stg = [AR.f32(KC * 768) for _ in range(2)]
        modf = AR.f32(96)
        bada = AR.f32(48)
        nm = AR.f32(16)
        modt = AR.f32(128)
        P.dma("pool", bada, I["b_ada"][l], w=["bada"])
        P.dma("pool", nm[:, 0:8], I["nmix"][l], w=["nm"])
        P.dma("pool", nm[:, 8:16], I["nffn"][l], w=["nm"])
        scs3 = scs.rearrange("p (k w) -> p k w", w=2)
        for cbk in range(8):
            s3 = stg[cbk % 2].rearrange("p (k n) -> p k n", k=KC)
            for kc in range(KC):
                P.dma("sp" if kc % 2 == 0 else "pool", s3[:, kc, :], I["w_ada"][l, kc * 128:(kc + 1) * 128, cbk * 768:(cbk + 1) * 768], w=[("stg", cbk % 2, kc)])
            for jj in range(6):
                j = cbk * 6 + jj
                P.mm([(ps[0][:, j * 2:j * 2 + 2], s3[:, kc, jj * 128:(jj + 1) * 128], scs3[:, kc, :], kc == 0, kc == KC - 1) for kc in range(KC)],
                     r=[("stg", cbk % 2, kc) for kc in range(KC)] + ["scs"], w=["ps0"])
        mf3 = modf.rearrange("p (j w) -> p j w", w=2)
        P.tt("dve", mf3, ps[0][:, 0:96].rearrange("p (j w) -> p j w", w=2), bada.unsqueeze(2).to_broadcast([128, 48, 2]), ALU.add,
             r=["ps0", "bada"], w=["modf"])
        nm3 = nm.rearrange("p (a k) -> p a k", a=2)
        for (Aap, Bap, jsc, jsh, a) in ((A1, B1, 8, 0, 0), (A2, B2, 32, 24, 1)):
            A3 = Aap.rearrange("p (k w) -> p k w", w=2)
            B3 = Bap.rearrange("p (k w) -> p k w", w=2)
            P.ts("dve", A3, mf3[:, jsc:jsc + 8, :], 1.0, None, op0=ALU.add, r=["modf"], w=[("A", a)])
            P.tt("dve", A3, A3, nm3[:, a, :].unsqueeze(2).to_broadcast([128, 8, 2]), ALU.mult, r=[("A", a), "nm"], w=[("A", a)])
            P.cp("dve", B3, mf3[:, jsh:jsh + 8, :], r=["modf"], w=[("B", a)])
        P.tr([(ps[1][0:96, 0:128], modf, ident)], r=["modf", "ident"], w=["ps1"])
        P.cp("act", modt[0:96, :], ps[1][0:96, 0:128], r=["ps1"], w=["modt"])
        P.dma("sp", MODD, modt[0:96, :], r=["modt"], w=["MODD"])
        md = MODD.rearrange("(j w) p -> w j p", w=2)
        for gi, (j0, wv) in enumerate(((16, 0), (16, 1), (40, 0), (40, 1))):
            src = md[wv, j0:j0 + 8, :]
            P.dma("sp", GB[:, gi * D:(gi + 1) * D].rearrange("p (j q) -> p j q", q=128), src.partition_broadcast(128), r=["MODD"], w=["GB"])
        AR.reset(m)
        P.barrier()

    def norm_tile(xt, sq, col, eng_sq="act"):
        P.actv(sq, xt, AF.Square, r=["xt"], w=["sq", "col"], accum_out=col[:, 0:1])
        P.actv(col[:, 1:2], col[:, 0:1], AF.Sqrt, r=["col"], w=["col1"], bias=EPS, scale=1.0 / D)
        P.recip(col[:, 2:3], col[:, 1:2], r=["col1"], w=["col2"])

    def phase_inproj(l):
        m = AR.mark()
        win = AR.bf16(KC * DIN)
        win3 = win.rearrange("p (k n) -> p k n", k=KC)
        stg = [AR.f32(DIN) for _ in range(2)]
        for kc in range(KC):
            s = stg[kc % 2]
            P.dma("pool", s, I["w_in"][l, kc * 128:(kc + 1) * 128, :], w=[("stg", kc % 2)])
            P.cp("pool", win3[:, kc, :], s, r=[("stg", kc % 2)], w=["win"])
        xt = AR.f32(D)
        sq = AR.f32(D)
        col = AR.f32(4)
        xn = AR.bf16(D)
        hT = AR.bf16(D)
        prj = AR.f32(DIN)
        hT3 = hT.rearrange("p (k t) -> p k t", k=KC)
        for t in range(NT):
            w = 0 if t < NTL else 1
            P.dma("sp", xt, X[t * 128:(t + 1) * 128, :], r=["X"], w=["xt"])
            norm_tile(xt, sq, col)
            P.ts("dve", xn, xt, col[:, 2:3], None, op0=ALU.mult, r=["xt", "col2"], w=["xn"])
            pb = psb(0)
            P.tr([(pb[:, k * 128:(k + 1) * 128], xn[:, k * 128:(k + 1) * 128], identb) for k in range(KC)], r=["xn", "identb"], w=["ps0"])
            A3 = A1.rearrange("p (k w) -> p k w", w=2)
            B3 = B1.rearrange("p (k w) -> p k w", w=2)
            for k in range(KC):
                P.actv(hT3[:, k, :], pb[:, k * 128:(k + 1) * 128], AF.Identity, r=["ps0", ("A", 0), ("B", 0)], w=[("hT", k)],
                       bias=B3[:, k, w:w + 1], scale=A3[:, k, w:w + 1])
            for cb in range(6):
                c0 = cb * 512
                c1 = min(DIN, c0 + 512)
                bank = 1 + (cb % 3)
                P.mm([(ps[bank][:, 0:c1 - c0], hT3[:, k, :], win3[:, k, c0:c1], k == 0, k == KC - 1) for k in range(KC)],
                     r=[("hT", k) for k in range(KC)] + ["win"], w=[("ps", bank)])
                P.cp("dve" if cb % 2 == 0 else "act", prj[:, c0:c1], ps[bank][:, 0:c1 - c0], r=[("ps", bank)], w=[("prj", cb)])
            P.dma("sp", PROJ[t * 128:(t + 1) * 128, :], prj, r=[("prj", cb) for cb in range(6)], w=["PROJ"])
        AR.reset(m)
        P.barrier()
        if dbg and l == 0:
            P.dma("sp", DBG["PROJ"], PROJ, r=["PROJ"], w=["dPROJ"])
            P.barrier()

    def attention(qT, kT, V, nqb, QB, ktiles, vslot, scale, dqk, sink, tag):
        for qb in range(nqb):
            nk = len(ktiles)
            for i, kt in enumerate(ktiles):
                sb = 1 + (i % 2)
                P.mm([(ps[sb][:, 0:QB], kT(kt), qT(qb), True, True)], r=[(tag + "k", kt), (tag + "q", qb)], w=[("ps", sb)])
                pt = PT[i % 2]
                P.actv(pt[:, 0:QB], ps[sb][:, 0:QB], AF.Exp, r=[("ps", sb)], w=[("pt", i % 2)], scale=scale)
                P.mm([(ps[3][0:65, 0:QB], V(kt), pt[:, 0:QB], i == 0, i == nk - 1)], r=[("pt", i % 2), (tag + "v", kt)], w=[("ps", 3)])
            P.cp("dve", OT[0:65, 0:QB], ps[3][0:65, 0:QB], r=[("ps", 3)], w=["ot"])
            for sub in range(QB // 128):
                P.tr([(ps[4][:, 0:65], OT[0:65, sub * 128:(sub + 1) * 128], ident[0:65, 0:65])], r=["ot", "ident"], w=[("ps", 4)])
                sink(qb, sub, ps[4][:, 0:65])

    def phase_attn(l, need_ctx, lam_init):
        nonlocal PT, OT
        m = AR.mark()
        B1buf = AR.bf16(8 * T)
        B2buf = AR.bf16(8 * T)
        VB = AR.bf16(NT * 4 * 65)
        PT = [AR.bf16(512) for _ in range(2)]
        OT = AR.f32(512)
        pin = AR.f32(768)
        tmp = AR.f32(768)
        tmp2 = AR.f32(768)
        qkb = AR.bf16(768)
        col = AR.f32(16)
        gqk = AR.f32(384)
        rop = AR.f32(64)
        ores = AR.f32(64)
        P.dma("pool", gqk, I["gqk"][l, 0].partition_broadcast(128), w=["gqk"])
        QB = min(512, L)
        nqb = L // QB
        VB4 = VB.rearrange("p (t h e) -> p t h e", t=NT, h=4)
        P.ms("pool", VB, 1.0, w=["VB"])
        P.barrier()
        QT = B1buf[0:64, 0:4 * L].rearrange("p (h t) -> p h t", h=4)
        KT = B2buf[0:64, 0:2 * T].rearrange("p (h t) -> p h t", h=2)
        QTC = B1buf[0:64, 4 * L:4 * L + 4 * LC].rearrange("p (h t) -> p h t", h=4)
        for t in range(NT):
            lat = t < NTL
            P.dma("sp", pin[:, 0:512], PROJ[t * 128:(t + 1) * 128, 0:512], w=["pin"])
            if lat:
                P.dma("sp", rop, I["ropa"][t * 128:(t + 1) * 128, :], w=["rop"])
            q3 = pin[:, 0:384].rearrange("p (h d) -> p h d", h=6)
            t3 = tmp[:, 0:384].rearrange("p (h d) -> p h d", h=6)
            u3 = tmp2[:, 0:384].rearrange("p (h d) -> p h d", h=6)
            P.tt("dve", t3, q3, q3, ALU.mult, r=["pin"], w=["tmp"])
            P.red("dve", col[:, 0:6], t3, ALU.add, r=["tmp"], w=["col"])
            P.actv(col[:, 6:12], col[:, 0:6], AF.Sqrt, r=["col"], w=["colb"], bias=EPS, scale=1.0 / 64)
            P.recip(col[:, 0:6], col[:, 6:12], r=["colb"], w=["col"])
            P.tt("dve", t3, q3, col[:, 0:6].unsqueeze(2).to_broadcast([128, 6, 64]), ALU.mult, r=["pin", "col"], w=["tmp"])
            P.tt("dve", tmp[:, 0:384], tmp[:, 0:384], gqk, ALU.mult, r=["tmp", "gqk"], w=["tmp"])
            qb3 = qkb[:, 0:384].rearrange("p (h d) -> p h d", h=6)
            if lat:
                cs = rop[:, 0:32].unsqueeze(1).to_broadcast([128, 6, 32])
                sn = rop[:, 32:64].unsqueeze(1).to_broadcast([128, 6, 32])
                x1 = t3[:, :, 0:32]
                x2 = t3[:, :, 32:64]
                P.tt("dve", u3[:, :, 0:32], x1, cs, ALU.mult, r=["tmp", "rop"], w=["tmp2a"])
                P.tt("pool", u3[:, :, 32:64], x2, sn, ALU.mult, r=["tmp", "rop"], w=["tmp2b"])
                P.tt("dve", qb3[:, :, 0:32], u3[:, :, 0:32], u3[:, :, 32:64], ALU.subtract, r=["tmp2a", "tmp2b"], w=["qkb"])
                P.tt("dve", u3[:, :, 0:32], x1, sn, ALU.mult, r=["tmp", "rop", "qkb"], w=["tmp2a"])
                P.tt("pool", u3[:, :, 32:64], x2, cs, ALU.mult, r=["tmp", "rop", "qkb"], w=["tmp2b"])
                P.tt("dve", qb3[:, :, 32:64], u3[:, :, 0:32], u3[:, :, 32:64], ALU.add, r=["tmp2a", "tmp2b"], w=["qkb"])
            else:
                P.cp("dve", qkb[:, 0:384], tmp[:, 0:384], r=["tmp"], w=["qkb"])
            pb = psb(0)
            P.tr([(pb[0:64, h * 128:(h + 1) * 128], qkb[:, h * 64:(h + 1) * 64], identb) for h in range(6)], r=["qkb", "identb"], w=[("ps", 0)])
            pb3 = pb[0:64, 0:768].rearrange("p (h t) -> p h t", h=6)
            if lat:
                P.cp("act", QT[:, :, t * 128:(t + 1) * 128], pb3[:, 0:4, :], r=[("ps", 0)], w=[("gq", t * 128 // QB)])
            else:
                tc_ = t - NTL
                P.cp("act", QTC[:, :, tc_ * 128:(tc_ + 1) * 128], pb3[:, 0:4, :], r=[("ps", 0)], w=[("gqc", 0)])
            P.cp("act", KT[:, :, t * 128:(t + 1) * 128], pb3[:, 4:6, :], r=[("ps", 0)], w=[("gk", t)])
            P.cp("pool", VB4[:, t, 0:2, 0:64], pin[:, 384:512].rearrange("p (h d) -> p h d", h=2), r=["pin"], w=[("gv", t)])
        P.barrier()
        for h in range(4):
            hk = h // 2

            def sink(qb, sub, res, h=h):
                tok = qb * QB + sub * 128
                P.recip(col[:, 12:13], res[:, 64:65], r=[("ps", 4)], w=["colr"])
                P.ts("dve", ores, res[:, 0:64], col[:, 12:13], None, op0=ALU.mult, r=[("ps", 4), "colr"], w=["ores"])
                P.dma("pool", CAT[tok:tok + 128, h * 64:(h + 1) * 64], ores, r=["ores"], w=[("CATa", h, tok)])
            attention(lambda qb, h=h: QT[:, h, qb * QB:(qb + 1) * QB], lambda kt, hk=hk: KT[:, hk, kt * 128:(kt + 1) * 128],
                      lambda kt, hk=hk: VB4[:, kt, hk, :], nqb, QB, list(range(NT)), None, 0.125, 64, sink, "g")
            if need_ctx:
                def sinkc(qb, sub, res, h=h):
                    tok = L + sub * 128
                    P.recip(col[:, 12:13], res[:, 64:65], r=[("ps", 4)], w=["colr"])
                    P.ts("dve", ores, res[:, 0:64], col[:, 12:13], None, op0=ALU.mult, r=[("ps", 4), "colr"], w=["ores"])
                    P.dma("pool", CAT[tok:tok + 128, h * 64:(h + 1) * 64], ores, r=["ores"], w=[("CATa", h, tok)])
                attention(lambda qb, h=h: QTC[:, h, 0:LC], lambda kt, hk=hk: KT[:, hk, kt * 128:(kt + 1) * 128],
                          lambda kt, hk=hk: VB4[:, kt, hk, :], 1, LC, list(range(NTL, NT)), None, 0.125, 64, sinkc, "gc")
        P.barrier()
        DQ = B1buf[0:32, 0:8 * L].rearrange("p (h t) -> p h t", h=8)
        DK = B2buf[0:32, 0:8 * T].rearrange("p (h t) -> p h t", h=8)
        DQC = B1buf[0:32, 8 * L:8 * L + 8 * LC].rearrange("p (h t) -> p h t", h=8)
        O1 = AR.f32(NT * 64)
        lamt = AR.f32(128)
        subl = AR.f32(64)
        P.dma("pool", lamt, I["lam"][l, 0].partition_broadcast(128), w=["lamt"])
        P.dma("pool", subl, I["subln"][l, 0].partition_broadcast(128), w=["subl"])
        l4 = lamt.rearrange("p (a d) -> p a d", a=4)
        lt = AR.f32(64)
        lt3 = lt.rearrange("p (a d) -> p a d", a=2)
        P.tt("dve", lt3[:, 0, :], l4[:, 0, :], l4[:, 1, :], ALU.mult, r=["lamt"], w=["lt"])
        P.tt("dve", lt3[:, 1, :], l4[:, 2, :], l4[:, 3, :], ALU.mult, r=["lamt", "lt"], w=["lt"])
        lc = AR.f32(8)
        P.red("dve", lc[:, 0:2], lt3, ALU.add, r=["lt"], w=["lc"])
        P.actv(lc[:, 2:4], lc[:, 0:2], AF.Exp, r=["lc"], w=["lc2"])
        P.tt("dve", lc[:, 4:5], lc[:, 3:4], lc[:, 2:3], ALU.subtract, r=["lc2"], w=["lc4"])
        P.ts("dve", lc[:, 5:6], lc[:, 4:5], -float(lam_init), None, op0=ALU.add, r=["lc4"], w=["nlam"])
        for t in range(NT):
            lat = t < NTL
            P.dma("sp", pin, PROJ[t * 128:(t + 1) * 128, 1024:1792], w=["pin"])
            qsrc = pin[:, 0:512]
            if lat:
                P.dma("sp", rop[:, 0:32], I["ropd"][t * 128:(t + 1) * 128, :], w=["rop"])
                s3 = pin[:, 0:512].rearrange("p (h d) -> p h d", h=16)
                u3 = tmp[:, 0:512].rearrange("p (h d) -> p h d", h=16)
                v3 = tmp2[:, 0:512].rearrange("p (h d) -> p h d", h=16)
                qb3 = qkb[:, 0:512].rearrange("p (h d) -> p h d", h=16)
                cs = rop[:, 0:16].unsqueeze(1).to_broadcast([128, 16, 16])
                sn = rop[:, 16:32].unsqueeze(1).to_broadcast([128, 16, 16])
                x1 = s3[:, :, 0:16]
                x2 = s3[:, :, 16:32]
                P.tt("dve", u3[:, :, 0:16], x1, cs, ALU.mult, r=["pin", "rop"], w=["tmpa"])
                P.tt("pool", u3[:, :, 16:32], x2, sn, ALU.mult, r=["pin", "rop"], w=["tmpb"])
                P.tt("dve", qb3[:, :, 0:16], u3[:, :, 0:16], u3[:, :, 16:32], ALU.subtract, r=["tmpa", "tmpb"], w=["qkb"])
                P.tt("dve", v3[:, :, 0:16], x1, sn, ALU.mult, r=["pin", "rop"], w=["tmp2a"])
                P.tt("pool", v3[:, :, 16:32], x2, cs, ALU.mult, r=["pin", "rop"], w=["tmp2b"])
                P.tt("dve", qb3[:, :, 16:32], v3[:, :, 0:16], v3[:, :, 16:32], ALU.add, r=["tmp2a", "tmp2b"], w=["qkb"])
            else:
                P.cp("dve", qkb[:, 0:512], pin[:, 0:512], r=["pin"], w=["qkb"])
            for half in range(2):
                bank = half
                pb = psb(bank)
                P.tr([(pb[0:32, mI * 128:(mI + 1) * 128], qkb[:, half * 256 + mI * 32:half * 256 + (mI + 1) * 32], identb) for mI in range(8)],
                     r=["qkb", "identb"], w=[("ps", bank)])
                pb3 = pb[0:32, 0:1024].rearrange("p (h t) -> p h t", h=8)
                if half == 0:
                    if lat:
                        P.cp("act", DQ[:, :, t * 128:(t + 1) * 128], pb3, r=[("ps", bank)], w=[("dq", t * 128 // QB)])
                    else:
                        tc_ = t - NTL
                        P.cp("act", DQC[:, :, tc_ * 128:(tc_ + 1) * 128], pb3, r=[("ps", bank)], w=[("dqc", 0)])
                else:
                    P.cp("act", DK[:, :, t * 128:(t + 1) * 128], pb3, r=[("ps", bank)], w=[("dk", t)])
            P.cp("pool", VB4[:, t, :, 0:64], pin[:, 512:768].rearrange("p (h d) -> p h d", h=4), r=["pin"], w=[("dv", t)])
        O13 = O1.rearrange("p (t d) -> p t d", d=64)
        dscale = 32 ** -0.5
        P.barrier()

        def finish(res_tile, tok, h):
            P.tt("dve", tmp2[:, 0:64], tmp[:, 0:64], tmp[:, 0:64], ALU.mult, r=["od"], w=["od2"])
            P.red("dve", col[:, 0:1], tmp2[:, 0:64], ALU.add, r=["od2"], w=["c0"])
            P.actv(col[:, 1:2], col[:, 0:1], AF.Sqrt, r=["c0"], w=["c1"], bias=EPS, scale=1.0 / 64)
            P.recip(col[:, 2:3], col[:, 1:2], r=["c1"], w=["c2"])
            P.stt("dve", ores, tmp[:, 0:64], col[:, 2:3], subl, ALU.mult, ALU.mult, r=["od", "c2", "subl"], w=["ores"])
            P.ts("dve", ores, ores, 1.0 - float(lam_init), None, op0=ALU.mult, r=["ores"], w=["ores"])
            P.dma("pool", CAT[tok:tok + 128, 512 + h * 64:512 + (h + 1) * 64], ores, r=["ores"], w=[("CATc", h, tok)])

        for h in range(4):
            for mI in range(2):
                hm = 2 * h + mI

                def sink(qb, sub, res, h=h, mI=mI, base=0, QBv=QB):
                    tok = base + qb * QBv + sub * 128
                    tI = tok // 128
                    P.recip(col[:, 12:13], res[:, 64:65], r=[("ps", 4)], w=["colr"])
                    if mI == 0:
                        P.ts("dve", O13[:, tI, :], res[:, 0:64], col[:, 12:13], None, op0=ALU.mult, r=[("ps", 4), "colr"], w=[("o1", tI)])
                    else:
                        P.ts("dve", tmp2[:, 64:128], res[:, 0:64], col[:, 12:13], None, op0=ALU.mult, r=[("ps", 4), "colr"], w=["o2"])
                        P.stt("dve", tmp[:, 0:64], tmp2[:, 64:128], lc[:, 5:6], O13[:, tI, :], ALU.mult, ALU.add, r=["o2", "nlam", ("o1", tI)], w=["od"])
                        finish(None, tok, h)
                attention(lambda qb, hm=hm: DQ[:, hm, qb * QB:(qb + 1) * QB], lambda kt, hm=hm: DK[:, hm, kt * 128:(kt + 1) * 128],
                          lambda kt, h=h: VB4[:, kt, h, :], nqb, QB, list(range(NT)), None, dscale, 32, sink, "d")
                if need_ctx:
                    attention(lambda qb, hm=hm: DQC[:, hm, 0:LC], lambda kt, hm=hm: DK[:, hm, kt * 128:(kt + 1) * 128],
                              lambda kt, h=h: VB4[:, kt, h, :], 1, LC, list(range(NTL, NT)), None, dscale, 32,
                              lambda qb, sub, res, s=sink: s(qb, sub, res, base=L, QBv=LC), "dc")
        AR.reset(m)
        P.barrier()

    PT = None
    OT = None

    def gelu_tanh(dst, src, t1, t2, n, tagr):
        P.tt("dve", t1, src, src, ALU.mult, r=tagr, w=["g_t1"])
        P.ts("dve", t1, t1, 0.044715, 1.0, op0=ALU.mult, op1=ALU.add, r=["g_t1"], w=["g_t1"])
        P.tt("dve", t1, t1, src, ALU.mult, r=["g_t1"] + tagr, w=["g_t1"])
        P.actv(t2, t1, AF.Sigmoid, r=["g_t1"], w=["g_t2"], scale=1.5957691216057308)
        P.tt("dve", dst, t2, src, ALU.mult, r=["g_t2"] + tagr, w=["g_dst"])

    def phase_gmlp(l, need_ctx):
        m = AR.mark()
        wsT = AR.f32(512)
        wst = AR.f32(128)
        bs = AR.f32(4)
        gv = AR.f32(256)
        pin = AR.f32(512)
        t1 = AR.f32(512)
        t2 = AR.f32(512)
        ge = AR.f32(512)
        vn = AR.f32(256)
        ob = AR.f32(256)
        col = AR.f32(4)
        P.dma("pool", bs, I["b_s"][l], w=["bs"])
        P.dma("pool", gv, I["gvn"][l, 0].partition_broadcast(128), w=["gv"])
        for g in range(4):
            P.dma("sp", wst, I["w_s"][l, g], w=["wst"])
            P.tr([(ps[0][:, 0:128], wst, ident)], r=["wst", "ident"], w=["ps0"])
            P.cp("dve", wsT[:, g * 128:(g + 1) * 128], ps[0][:, 0:128], r=["ps0"], w=["wsT"])
        ntiles = NT if need_ctx else NTL
        for t in range(ntiles):
            P.dma("sp", pin, PROJ[t * 128:(t + 1) * 128, 512:1024], w=["pin"])
            gelu_tanh(ge, pin, t1, t2, 512, ["pin"])
            P.tt("dve", t1[:, 0:256], ge[:, 256:512], ge[:, 256:512], ALU.mult, r=["g_dst"], w=["g_t1"])
            P.red("dve", col[:, 0:1], t1[:, 0:256], ALU.add, r=["g_t1"], w=["c0"])
            P.actv(col[:, 1:2], col[:, 0:1], AF.Sqrt, r=["c0"], w=["c1"], bias=EPS, scale=1.0 / 256)
            P.recip(col[:, 2:3], col[:, 1:2], r=["c1"], w=["c2"])
            P.stt("dve", vn, ge[:, 256:512], col[:, 2:3], gv, ALU.mult, ALU.mult, r=["g_dst", "c2", "gv"], w=["vn"])
            P.mm([(ps[1][:, g * 64:(g + 1) * 64], wsT[:, g * 128:(g + 1) * 128], vn[:, g * 64:(g + 1) * 64], True, True) for g in range(4)],
                 r=["vn", "wsT"], w=["ps1"])
            for g in range(4):
                P.stt("dve", ob[:, g * 64:(g + 1) * 64], ps[1][:, g * 64:(g + 1) * 64], bs[:, g:g + 1], ge[:, g * 64:(g + 1) * 64], ALU.add, ALU.mult,
                      r=["ps1", "bs", "g_dst"], w=["ob"])
            P.dma("pool", CAT[t * 128:(t + 1) * 128, 256:512], ob, r=["ob"], w=[("CATb", t)])
        AR.reset(m)
        P.barrier()

    def phase_gdn(l, need_ctx):
        m = AR.mark()
        cw = AR.f32(3 * 768)
        alg = AR.f32(16)
        xp = AR.f32(768)
        xc = AR.f32(768)
        xnx = AR.f32(768)
        y = AR.f32(768)
        y2 = AR.f32(768)
        gp = AR.f32(784)
        ba = AR.f32(16)
        col = AR.f32(16)
        for k in range(3):
            P.dma("pool", cw[:, k * 768:(k + 1) * 768], I["convw"][l, k].partition_broadcast(128), w=["cw"])
        P.dma("pool", alg[:, 0:8], I["alog"][l, 0].partition_broadcast(128), w=["alg"])
        P.dma("pool", alg[:, 8:16], I["dtb"][l, 0].partition_broadcast(128), w=["alg"])
        P.actv(alg[:, 0:8], alg[:, 0:8], AF.Exp, r=["alg"], w=["alg"])
        c0 = OFF["dqkv"]
        for t in range(NT):
            seq0 = 0 if t < NTL else L
            seq1 = L if t < NTL else T
            r0 = t * 128
            P.dma("sp", xc, PROJ[r0:r0 + 128, c0:c0 + 768], w=["xc"])
            P.dma("sp", ba, PROJ[r0:r0 + 128, OFF["db"]:OFF["db"] + 16], w=["ba"])
            if r0 == seq0:
                P.ms("pool", xp[0:1, :], 0.0, w=["xp"])
                P.dma("sp", xp[1:128, :], PROJ[r0:r0 + 127, c0:c0 + 768], w=["xp"])
            else:
                P.dma("sp", xp, PROJ[r0 - 1:r0 + 127, c0:c0 + 768], w=["xp"])
            if r0 + 128 == seq1:
                P.ms("pool", xnx, 0.0, w=["xnx"])
                P.dma("sp", xnx[0:127, :], PROJ[r0 + 1:r0 + 128, c0:c0 + 768], w=["xnx"])
            else:
                P.dma("sp", xnx, PROJ[r0 + 1:r0 + 129, c0:c0 + 768], w=["xnx"])
            P.tt("dve", y, xp, cw[:, 0:768], ALU.mult, r=["xp", "cw"], w=["y"])
            P.tt("pool", y2, xc, cw[:, 768:1536], ALU.mult, r=["xc", "cw"], w=["y2"])
            P.tt("dve", y, y, y2, ALU.add, r=["y", "y2"], w=["y"])
            P.tt("pool", y2, xnx, cw[:, 1536:2304], ALU.mult, r=["xnx", "cw", "y"], w=["y2"])
            P.tt("dve", y, y, y2, ALU.add, r=["y", "y2"], w=["y"])
            P.actv(y2, y, AF.Sigmoid, r=["y"], w=["y2"])
            P.tt("dve", y, y, y2, ALU.mult, r=["y", "y2"], w=["y"])
            y3 = y[:, 0:512].rearrange("p (h d) -> p h d", h=8)
            s3 = y2[:, 0:512].rearrange("p (h d) -> p h d", h=8)
            P.tt("dve", s3, y3, y3, ALU.mult, r=["y"], w=["y2"])
            P.red("dve", col[:, 0:8], s3, ALU.add, r=["y2"], w=["c0"])
            P.actv(col[:, 8:16], col[:, 0:8], AF.Sqrt, r=["c0"], w=["c1"], bias=EPS, scale=1.0)
            P.recip(col[:, 0:8], col[:, 8:16], r=["c1"], w=["c2"])
            P.ts("dve", col[:, 0:4], col[:, 0:4], 0.125, None, op0=ALU.mult, r=["c2"], w=["c2"])
            g3 = gp[:, 0:512].rearrange("p (h d) -> p h d", h=8)
            P.tt("dve", g3, y3, col[:, 0:8].unsqueeze(2).to_broadcast([128, 8, 64]), ALU.mult, r=["y", "c2"], w=["gp"])
            P.cp("pool", gp[:, 512:768], y[:, 512:768], r=["y"], w=["gpv"])
            P.actv(gp[:, 768:776], ba[:, 0:8], AF.Sigmoid, r=["ba"], w=["gpb"])
            P.tt("dve", ba[:, 8:16], ba[:, 8:16], alg[:, 8:16], ALU.add, r=["ba", "alg"], w=["ba"])
            P.actv(ba[:, 8:16], ba[:, 8:16], AF.Exp, r=["ba"], w=["ba"])
            P.actv(ba[:, 8:16], ba[:, 8:16], AF.Ln, r=["ba"], w=["ba"], bias=1.0)
            P.stt("dve", gp[:, 776:784], ba[:, 8:16], -1.0, alg[:, 0:8], ALU.mult, ALU.mult, r=["ba", "alg"], w=["gpg"])
            P.dma("sp", GDNP[r0:r0 + 128, :], gp, r=["gp", "gpv", "gpb", "gpg"], w=[("GDNP", t)])
        AR.reset(m)
        P.barrier()
        if phases is not None and "nosweep" in phases:
            return
        msk = AR.f32(5 * 64)
        ones64 = ones[0:64, 0:64]
        ST = AR.f32(256)
        ST3 = ST[0:64, :].rearrange("p (h d) -> p h d", h=4)
        cin = AR.f32(784)
        KTs = AR.f32(256)
        QTs = AR.f32(256)
        KBT = AR.f32(256)
        kb = AR.f32(256)
        cg = AR.f32(16)
        Rm = AR.f32(256)
        E = AR.f32(256)
        X1 = AR.f32(256)
        X2 = AR.f32(256)
        tm = AR.f32(256)
        Mb = [AR.f32(256) for _ in range(2)]
        MTb = [AR.f32(256) for _ in range(2)]
        QKT = AR.f32(256)
        yv = AR.f32(512)
        WT = AR.f32(256)
        QTT = AR.f32(256)
        qt_ = AR.f32(256)
        kd = AR.f32(256)
        nv = AR.f32(256)
        osb = AR.f32(256)
        egb = AR.f32(256)
        ofl = AR.f32(256)
        zt = AR.f32(256)
        on = AR.f32(64)
        col2 = AR.f32(16)
        P.dma("pool", on, I["onorm"][l, 0].partition_broadcast(128), w=["on"])
        order_f = list(range(L // 64, NCH)) + list(range(0, L // 64))
        order_b = list(range(NCH - 1, L // 64 - 1, -1)) + list(range(L // 64 - 1, -1, -1))
        for dr in range(2):
            P.dma("sp", msk[0:64, :], I["gmask"][dr].rearrange("p a f -> p (a f)"), w=["msk"])
            U = msk[0:64, 0:64]
            nMS = msk[0:64, 64:128].unsqueeze(1).to_broadcast([64, 4, 64])
            nMST = msk[0:64, 128:192].unsqueeze(1).to_broadcast([64, 4, 64])
            MIT = msk[0:64, 192:256].unsqueeze(1).to_broadcast([64, 4, 64])
            P.ms("dve", ST, 0.0, w=["ST"])
            last = 63 if dr == 0 else 0
            for n in (order_f if dr == 0 else order_b):
                r0 = n * 64
                lat = r0 < L
                c = cin[0:64, :]
                P.dma("sp", c, GDNP[r0:r0 + 64, :], r=[("GDNP", r0 // 128)], w=["cin"])
                q3 = c[:, 0:256].rearrange("p (h d) -> p h d", h=4)
                k3 = c[:, 256:512].rearrange("p (h d) -> p h d", h=4)
                v3 = c[:, 512:768].rearrange("p (h d) -> p h d", h=4)
                beta = c[:, 768 + 4 * dr:772 + 4 * dr]
                gg = c[:, 776 + 4 * dr:780 + 4 * dr]
                kb3 = kb[0:64, :].rearrange("p (h d) -> p h d", h=4)
                P.tt("dve", kb3, k3, beta.unsqueeze(2).to_broadcast([64, 4, 64]), ALU.mult, r=["cin"], w=["kb"])
                P.tr([(ps[0][0:64, h * 64:(h + 1) * 64], c[:, 256 + h * 64:256 + (h + 1) * 64], ident[0:64, 0:64]) for h in range(4)] +
                     [(ps[0][0:64, 256 + h * 64:256 + (h + 1) * 64], c[:, h * 64:(h + 1) * 64], ident[0:64, 0:64]) for h in range(4)],
                     r=["cin", "ident"], w=[("ps", 0)])
                P.cp("act", KTs[0:64, :], ps[0][0:64, 0:256], r=[("ps", 0)], w=["KTs"])
                P.cp("act", QTs[0:64, :], ps[0][0:64, 256:512], r=[("ps", 0)], w=["QTs"])
                P.tr([(ps[1][0:64, h * 64:(h + 1) * 64], kb[0:64, h * 64:(h + 1) * 64], ident[0:64, 0:64]) for h in range(4)], r=["kb", "ident"], w=[("ps", 1)])
                P.mm([(ps[1][0:64, 256:260], U, gg, True, True)], r=["msk", "cin"], w=[("ps", 1)])
                P.cp("act", KBT[0:64, :], ps[1][0:64, 0:256], r=[("ps", 1)], w=["KBT"])
                P.cp("dve", cg[0:64, 0:4], ps[1][0:64, 256:260], r=[("ps", 1)], w=["cgc"])
                R3 = Rm[0:64, :].rearrange("p (h d) -> p h d", h=4)
                P.tt("dve", R3, gg.unsqueeze(2).to_broadcast([64, 4, 64]), U.unsqueeze(1).to_broadcast([64, 4, 64]), ALU.mult, r=["cin", "msk"], w=["Rm"])
                P.mm([(ps[2][0:64, 0:256], ones64, Rm[0:64, :], True, True)], r=["Rm", "ones"], w=[("ps", 2)])
                B3 = ps[2][0:64, 0:256].rearrange("p (h d) -> p h d", h=4)
                E3 = E[0:64, :].rearrange("p (h d) -> p h d", h=4)
                P.tt("dve", E3, B3, cg[0:64, 0:4].unsqueeze(2).to_broadcast([64, 4, 64]), ALU.subtract, r=[("ps", 2), "cgc"], w=["E"])
                P.tt("dve", cg[0:64, 8:12], B3[:, :, last], cg[0:64, 0:4], ALU.subtract, r=[("ps", 2), "cgc"], w=["cg8"])
                P.actv(cg[0:64, 8:12], cg[0:64, 8:12], AF.Exp, r=["cg8"], w=["cg8"])
                P.actv(cg[0:64, 12:16], B3[:, :, last], AF.Exp, r=[("ps", 2)], w=["cg12"])
                P.actv(cg[0:64, 4:8], cg[0:64, 0:4], AF.Exp, r=["cgc"], w=["cg4"])
                P.ts("dve", X1[0:64, :], E[0:64, :], 0.0, None, op0=ALU.min, r=["E"], w=["X1"])
                P.ts("dve", X2[0:64, :], E[0:64, :], -1.0, 0.0, op0=ALU.mult, op1=ALU.min, r=["E"], w=["X2"])
                P.actv(X1[0:64, :], X1[0:64, :], AF.Exp, r=["X1"], w=["X1"])
                P.actv(X2[0:64, :], X2[0:64, :], AF.Exp, r=["X2"], w=["X2"])
                P.mm([(ps[3][0:64, h * 64:(h + 1) * 64], KBT[0:64, h * 64:(h + 1) * 64], KTs[0:64, h * 64:(h + 1) * 64], True, True) for h in range(4)] +
                     [(ps[3][0:64, 256 + h * 64:256 + (h + 1) * 64], KTs[0:64, h * 64:(h + 1) * 64], KBT[0:64, h * 64:(h + 1) * 64], True, True) for h in range(4)],
                     r=["KBT", "KTs"], w=[("ps", 3)])
                P.mm([(ps[4][0:64, h * 64:(h + 1) * 64], KTs[0:64, h * 64:(h + 1) * 64], QTs[0:64, h * 64:(h + 1) * 64], True, True) for h in range(4)],
                     r=["QTs", "KTs"], w=[("ps", 4)])
                M0 = Mb[0][0:64, :]
                MT0 = MTb[0][0:64, :]
                tm3 = tm[0:64, :].rearrange("p (h d) -> p h d", h=4)
                P.tt("dve", tm3, X2[0:64, :].rearrange("p (h d) -> p h d", h=4), nMS, ALU.mult, r=["X2", "msk"], w=["tm"])
                P.tt("dve", M0, ps[3][0:64, 0:256], tm[0:64, :], ALU.mult, r=[("ps", 3), "tm"], w=[("M", 0)])
                P.tt("dve", tm3, X1[0:64, :].rearrange("p (h d) -> p h d", h=4), nMST, ALU.mult, r=["X1", "msk", ("M", 0)], w=["tm"])
                P.tt("dve", MT0, ps[3][0:64, 256:512], tm[0:64, :], ALU.mult, r=[("ps", 3), "tm"], w=[("MT", 0)])
                P.tt("dve", tm3, X1[0:64, :].rearrange("p (h d) -> p h d", h=4), MIT, ALU.mult, r=["X1", "msk", ("MT", 0)], w=["tm"])
                P.tt("dve", QKT[0:64, :], ps[4][0:64, 0:256], tm[0:64, :], ALU.mult, r=[("ps", 4), "tm"], w=["QKT"])
                y4 = yv[0:64, :].rearrange("p (h e) -> p h e", h=4)
                P.tt("dve", y4[:, :, 0:64], v3, beta.unsqueeze(2).to_broadcast([64, 4, 64]), ALU.mult, r=["cin"], w=["yv"])
                P.tt("dve", y4[:, :, 64:128], kb3, cg[0:64, 4:8].unsqueeze(2).to_broadcast([64, 4, 64]), ALU.mult, r=["kb", "cg4"], w=["yv"])
                qt3 = qt_[0:64, :].rearrange("p (h d) -> p h d", h=4)
                kd3 = kd[0:64, :].rearrange("p (h d) -> p h d", h=4)
                P.tt("pool", qt3, q3, cg[0:64, 4:8].unsqueeze(2).to_broadcast([64, 4, 64]), ALU.mult, r=["cin", "cg4"], w=["qt"])
                P.tt("pool", kd3, k3, cg[0:64, 8:12].unsqueeze(2).to_broadcast([64, 4, 64]), ALU.mult, r=["cin", "cg8"], w=["kd"])
                P.tt("pool", egb[0:64, :].rearrange("p (h d) -> p h d", h=4), ones[0:64, 0:1].unsqueeze(2).to_broadcast([64, 4, 64]),
                     cg[0:64, 12:16].unsqueeze(2).to_broadcast([64, 4, 64]), ALU.mult, r=["ones", "cg12"], w=["egb"])
                P.tr([(ps[4][0:64, 256 + h * 64:256 + (h + 1) * 64], qt_[0:64, h * 64:(h + 1) * 64], ident[0:64, 0:64]) for h in range(4)],
                     r=["qt", "ident"], w=[("ps", 4)])
                P.cp("act", QTT[0:64, :], ps[4][0:64, 256:512], r=[("ps", 4)], w=["QTT"])
                for lv in range(6):
                    Mc = Mb[lv % 2][0:64, :]
                    MTc = MTb[lv % 2][0:64, :]
                    P.mm([(ps[5][0:64, h * 128:(h + 1) * 128], MTc[:, h * 64:(h + 1) * 64], yv[0:64, h * 128:(h + 1) * 128], True, True) for h in range(4)],
                         r=[("MT", lv % 2), "yv"], w=[("ps", 5)])
                    if lv < 5:
                        P.mm([(ps[6][0:64, h * 64:(h + 1) * 64], MTc[:, h * 64:(h + 1) * 64], Mc[:, h * 64:(h + 1) * 64], True, True) for h in range(4)] +
                             [(ps[6][0:64, 256 + h * 64:256 + (h + 1) * 64], Mc[:, h * 64:(h + 1) * 64], MTc[:, h * 64:(h + 1) * 64], True, True) for h in range(4)],
                             r=[("MT", lv % 2), ("M", lv % 2)], w=[("ps", 6)])
                    P.tt("dve", yv[0:64, :], yv[0:64, :], ps[5][0:64, :], ALU.add, r=["yv", ("ps", 5)], w=["yv"])
                    if lv < 5:
                        P.cp("act", Mb[(lv + 1) % 2][0:64, :], ps[6][0:64, 0:256], r=[("ps", 6)], w=[("M", (lv + 1) % 2)])
                        P.cp("act", MTb[(lv + 1) % 2][0:64, :], ps[6][0:64, 256:512], r=[("ps", 6)], w=[("MT", (lv + 1) % 2)])
                P.tr([(ps[6][0:64, h * 64:(h + 1) * 64], yv[0:64, h * 128 + 64:h * 128 + 128], ident[0:64, 0:64]) for h in range(4)], r=["yv", "ident"], w=[("ps", 6)])
                P.cp("act", WT[0:64, :], ps[6][0:64, 0:256], r=[("ps", 6)], w=["WT"])
                P.mm([(ps[7][0:64, h * 64:(h + 1) * 64], WT[0:64, h * 64:(h + 1) * 64], ST[0:64, h * 64:(h + 1) * 64], True, True) for h in range(4)],
                     r=["WT", "ST"], w=[("ps", 7)])
                nv3 = nv[0:64, :].rearrange("p (h d) -> p h d", h=4)
                P.tt("dve", nv3, y4[:, :, 0:64], ps[7][0:64, 0:256].rearrange("p (h d) -> p h d", h=4), ALU.subtract, r=["yv", ("ps", 7)], w=["nv"])
                items = []
                for h in range(4):
                    items.append((ps[7][0:64, 256 + h * 64:256 + (h + 1) * 64], QTT[0:64, h * 64:(h + 1) * 64], ST[0:64, h * 64:(h + 1) * 64], True, False))
                    items.append((ps[7][0:64, 256 + h * 64:256 + (h + 1) * 64], QKT[0:64, h * 64:(h + 1) * 64], nv[0:64, h * 64:(h + 1) * 64], False, True))
                P.mm(items, r=["QTT", "ST", "QKT", "nv"], w=[("ps", 7)])
                P.mm([(ps[2][0:64, 256 + h * 64:256 + (h + 1) * 64], kd[0:64, h * 64:(h + 1) * 64], nv[0:64, h * 64:(h + 1) * 64], True, True) for h in range(4)],
                     r=["kd", "nv"], w=[("ps", 2)])
                P.tt("dve", ST[0:64, :], ST[0:64, :], egb[0:64, :], ALU.mult, r=["ST", "egb"], w=["ST"])
                P.tt("dve", ST[0:64, :], ST[0:64, :], ps[2][0:64, 256:512], ALU.add, r=["ST", ("ps", 2)], w=["ST"])
                need_o = lat or need_ctx
                if need_o:
                    if dr == 0:
                        P.cp("act", osb[0:64, :], ps[7][0:64, 256:512], r=[("ps", 7)], w=["osb"])
                        P.dma("sp", OF[r0:r0 + 64, :], osb[0:64, :], r=["osb"], w=[("OF", n)])
                    else:
                        P.dma("sp", ofl[0:64, :], OF[r0:r0 + 64, :], r=[("OF", n)], w=["ofl"])
                        P.dma("sp", zt[0:64, :], PROJ[r0:r0 + 64, OFF["dz"]:OFF["dz"] + 256], w=["zt"])
                        P.tt("dve", osb[0:64, :], ofl[0:64, :], ps[7][0:64, 256:512], ALU.add, r=["ofl", ("ps", 7)], w=["osb"])
                        o3 = osb[0:64, :].rearrange("p (h d) -> p h d", h=4)
                        s3 = ofl[0:64, :].rearrange("p (h d) -> p h d", h=4)
                        P.tt("dve", s3, o3, o3, ALU.mult, r=["osb"], w=["ofl"])
                        P.red("dve", col2[0:64, 0:4], s3, ALU.add, r=["ofl"], w=["c0"])
                        P.actv(col2[0:64, 4:8], col2[0:64, 0:4], AF.Sqrt, r=["c0"], w=["c1"], bias=EPS, scale=1.0 / 64)
                        P.recip(col2[0:64, 0:4], col2[0:64, 4:8], r=["c1"], w=["c2"])
                        P.tt("dve", o3, o3, col2[0:64, 0:4].unsqueeze(2).to_broadcast([64, 4, 64]), ALU.mult, r=["osb", "c2"], w=["osb"])
                        P.tt("dve", o3, o3, on[0:64, :].unsqueeze(1).to_broadcast([64, 4, 64]), ALU.mult, r=["osb", "on"], w=["osb"])
                        P.actv(ofl[0:64, :], zt[0:64, :], AF.Sigmoid, r=["zt", "ofl"], w=["ofl"])
                        P.tt("dve", zt[0:64, :], zt[0:64, :], ofl[0:64, :], ALU.mult, r=["zt", "ofl"], w=["zt"])
                        P.tt("dve", osb[0:64, :], osb[0:64, :], zt[0:64, :], ALU.mult, r=["osb", "zt"], w=["osb"])
                        P.dma("sp", CAT[r0:r0 + 64, 768:1024], osb[0:64, :], r=["osb"], w=[("CATd", n)])
            P.barrier()
        AR.reset(m)
        P.barrier()

    def phase_outproj(l, need_ctx):
        m = AR.mark()
        wo = AR.bf16(KC * D)
        wo3 = wo.rearrange("p (k n) -> p k n", k=KC)
        stg = [AR.f32(D) for _ in range(2)]
        for kc in range(KC):
            s = stg[kc % 2]
            P.dma("pool", s, I["w_out"][l, kc * 128:(kc + 1) * 128, :], w=[("stg", kc % 2)])
            P.cp("pool", wo3[:, kc, :], s, r=[("stg", kc % 2)], w=["wo"])
        ct = AR.f32(D)
        cb = AR.bf16(D)
        cT = AR.bf16(D)
        xt = AR.f32(D)
        yy = AR.f32(D)
        cT3 = cT.rearrange("p (k t) -> p k t", k=KC)
        ntiles = NT if need_ctx else NTL
        for t in range(ntiles):
            w = 0 if t < NTL else 1
            P.dma("sp", ct, CAT[t * 128:(t + 1) * 128, :], w=["ct"])
            P.dma("sp", xt, X[t * 128:(t + 1) * 128, :], r=[("X", t)], w=["xt"])
            P.cp("dve", cb, ct, r=["ct"], w=["cb"])
            pb = psb(0)
            P.tr([(pb[:, k * 128:(k + 1) * 128], cb[:, k * 128:(k + 1) * 128], identb) for k in range(KC)], r=["cb", "identb"], w=[("ps", 0)])
            P.cp("act", cT, pb[:, 0:1024], r=[("ps", 0)], w=["cT"])
            for hf in range(2):
                P.mm([(ps[1 + hf][:, 0:512], cT3[:, k, :], wo3[:, k, hf * 512:(hf + 1) * 512], k == 0, k == KC - 1) for k in range(KC)],
                     r=["cT", "wo"], w=[("ps", 1 + hf)])
                P.tt("dve", yy[:, hf * 512:(hf + 1) * 512], ps[1 + hf][:, 0:512], GB[:, w * D + hf * 512:w * D + (hf + 1) * 512], ALU.mult,
                     r=[("ps", 1 + hf), "GB"], w=[("yy", hf)])
                P.tt("pool", xt[:, hf * 512:(hf + 1) * 512], xt[:, hf * 512:(hf + 1) * 512], yy[:, hf * 512:(hf + 1) * 512], ALU.add,
                     r=["xt", ("yy", hf)], w=["xt"])
            P.dma("sp", X[t * 128:(t + 1) * 128, :], xt, r=["xt"], w=[("X", t)])
        AR.reset(m)
        P.barrier()
        if dbg and l == 0:
            P.dma("sp", DBG["CAT"], CAT, w=["dCAT"])
            P.dma("sp", DBG["X1"], X, w=["dX1"])
            P.barrier()

    def phase_ffn(l, need_ctx, final):
        m = AR.mark()
        ntiles = NT if need_ctx else NTL
        gates = AR.f32(NT * NE)
        gates3 = gates.rearrange("p (t e) -> p t e", e=NE)
        wr = AR.f32(KC * NE)
        wr3 = wr.rearrange("p (k e) -> p k e", k=KC)
        rb = AR.f32(NE)
        P.dma("pool", wr3, I["w_r"][l].rearrange("(k p) e -> p k e", p=128), w=["wr"])
        P.dma("pool", rb, I["b_r"][l, 0].partition_broadcast(128), w=["rb"])
        m2 = AR.mark()
        xt = AR.f32(D)
        sq = AR.f32(D)
        xn = AR.f32(D)
        fT = AR.f32(D)
        fTb = AR.bf16(D)
        col = AR.f32(16)
        lg = AR.f32(NE)
        ex = AR.f32(NE)
        top8 = AR.f32(8)
        fT3 = fT.rearrange("p (k t) -> p k t", k=KC)
        A3 = A2.rearrange("p (k w) -> p k w", w=2)
        B3 = B2.rearrange("p (k w) -> p k w", w=2)
        for t in range(ntiles):
            w = 0 if t < NTL else 1
            P.dma("sp", xt, X[t * 128:(t + 1) * 128, :], r=[("X", t)], w=["xt"])
            norm_tile(xt, sq, col)
            P.ts("dve", xn, xt, col[:, 2:3], None, op0=ALU.mult, r=["xt", "col2"], w=["xn"])
            for hf in range(2):
                P.tr([(ps[hf][:, k * 128:(k + 1) * 128], xn[:, (hf * 4 + k) * 128:(hf * 4 + k + 1) * 128], ident) for k in range(4)],
                     r=["xn", "ident"], w=[("ps", hf)])
                for k in range(4):
                    kk = hf * 4 + k
                    P.actv(fT3[:, kk, :], ps[hf][:, k * 128:(k + 1) * 128], AF.Identity, r=[("ps", hf), ("A", 1), ("B", 1)], w=[("fT", kk)],
                           bias=B3[:, kk, w:w + 1], scale=A3[:, kk, w:w + 1])
            P.cp("pool", fTb, fT, r=[("fT", k) for k in range(KC)], w=["fTb"])
            P.dma("pool", FTD[:, :, t * 128:(t + 1) * 128], fTb.rearrange("p (k t) -> p k t", k=KC), r=["fTb"], w=[("FTD", t)])
            P.mm([(ps[2][:, 0:NE], fT3[:, k, :], wr3[:, k, :], k == 0, k == KC - 1) for k in range(KC)], r=[("fT", k) for k in range(KC)] + ["wr"], w=[("ps", 2)])
            P.tt("dve", lg, ps[2][:, 0:NE], rb, ALU.add, r=[("ps", 2), "rb"], w=["lg"])
            P.add("dve", lambda e: e.max(out=top8, in_=lg), r=["lg"], w=["top8"])
            P.ts("dve", col[:, 4:5], top8[:, 0:1], -1.0, None, op0=ALU.mult, r=["top8"], w=["nmax"])
            P.actv(ex, lg, AF.Exp, r=["lg", "nmax"], w=["ex"], bias=col[:, 4:5])
            P.ts("dve", lg, lg, top8[:, TOPK - 1:TOPK], None, op0=ALU.is_ge, r=["lg", "top8"], w=["lg"])
            P.tt("dve", ex, ex, lg, ALU.mult, r=["ex", "lg"], w=["ex"])
            P.red("dve", col[:, 5:6], ex, ALU.add, r=["ex"], w=["gs"])
            P.recip(col[:, 6:7], col[:, 5:6], r=["gs"], w=["gr"])
            P.ts("dve", gates3[:, t, :], ex, col[:, 6:7], None, op0=ALU.mult, r=["ex", "gr"], w=[("gates", t)])
        AR.reset(m2)
        P.barrier()
        bdn = AR.f32(D)
        gT = AR.f32(128)
        bup = AR.f32(16)
        TG = 8
        groups = [list(range(g0, min(ntiles, g0 + TG))) for g0 in range(0, ntiles, TG)]
        fg = AR.bf16(KC * TG * 128)
        acc = AR.f32(TG * D)
        acc3 = acc.rearrange("p (t d) -> p t d", d=D)
        wu = [AR.bf16(KC * 2 * D) for _ in range(2)]
        wd = [AR.bf16(KC * D) for _ in range(1)]
        su = [AR.f32(D) for _ in range(3)]
        hid = [AR.bf16(KC * 512) for _ in range(2)]
        Gc = AR.f32(512)
        Sg = AR.f32(512)
        Uc = AR.f32(512)
        xt = AR.f32(D)
        sq = AR.f32(D)
        col = AR.f32(8)
        fn_ = AR.f32(D)
        if final:
            P.dma("pool", fn_, I["fnorm"][0].partition_broadcast(128), w=["fn"])
        P.dma("pool", bdn[0:NE, :], I["b_dn"][l], w=["bdn"])
        wcount = 0
        for gi, grp in enumerate(groups):
            ntg = len(grp)
            ntok = ntg * 128
            t0 = grp[0]
            fg3 = fg[:, 0:KC * ntok].rearrange("p (k t) -> p k t", k=KC)
            P.dma("sp", fg3, FTD[:, :, t0 * 128:t0 * 128 + ntok], r=[("FTD", t) for t in grp], w=["fg"])
            for ti, t in enumerate(grp):
                P.tr([(ps[7][0:NE, 0:128], gates3[:, t, :], ident)], r=[("gates", t), "ident"], w=[("ps", 7)])
                P.cp("act", gT[0:NE, :], ps[7][0:NE, 0:128], r=[("ps", 7)], w=["gT"])
                for hf in range(2):
                    P.mm([(ps[5 + hf][:, 0:512], gT[0:NE, :], bdn[0:NE, hf * 512:(hf + 1) * 512], True, True)], r=["gT", "bdn"], w=[("ps", 5 + hf)])
                    P.cp("dve", acc3[:, ti, hf * 512:(hf + 1) * 512], ps[5 + hf][:, 0:512], r=[("ps", 5 + hf)], w=[("acc", ti, hf)])
            blocks = [(b0, min(512, ntok - b0)) for b0 in range(0, ntok, 512)]
            for e in range(NE):
                par = wcount % 2
                wcount += 1
                wu3 = wu[par].rearrange("p (k n) -> p k n", k=KC)
                wd3 = wd[0].rearrange("p (k n) -> p k n", k=KC)
                sc_ = 0
                for kc in range(KC):
                    for hf in range(2):
                        s = su[sc_ % 3]
                        P.dma("sp", s, I["w_up"][l, e, kc * 128:(kc + 1) * 128, hf * D:(hf + 1) * D], w=[("su", sc_ % 3)])
                        P.cp("pool" if sc_ % 2 == 0 else "act", wu3[:, kc, hf * D:(hf + 1) * D], s, r=[("su", sc_ % 3)], w=[("wu", par, kc)])
                        sc_ += 1
                for kc in range(KC):
                    s = su[sc_ % 3]
                    P.dma("sp", s, I["w_dn"][l, e, kc * 128:(kc + 1) * 128, :], w=[("su", sc_ % 3)])
                    P.cp("pool" if sc_ % 2 == 0 else "act", wd3[:, kc, :], s, r=[("su", sc_ % 3)], w=[("wd", 0, kc)])
                    sc_ += 1
                P.dma("pool", bup, I["b_up"][l, e], w=["bup"])
                for bi, (b0, bn) in enumerate(blocks):
                    hb = hid[bi % 2]
                    hb3 = hb.rearrange("p (j t) -> p j t", j=KC)
                    for j in range(KC):
                        pg = (2 * j) % 4
                        pu = (2 * j + 1) % 4
                        P.mm([(ps[pg][:, 0:bn], wu3[:, k, j * 128:(j + 1) * 128], fg3[:, k, b0:b0 + bn], k == 0, k == KC - 1) for k in range(KC)],
                             r=[("wu", par, k) for k in range(KC)] + ["fg"], w=[("ps", pg)])
                        P.mm([(ps[pu][:, 0:bn], wu3[:, k, D + j * 128:D + (j + 1) * 128], fg3[:, k, b0:b0 + bn], k == 0, k == KC - 1) for k in range(KC)],
                             r=[("wu", par, k) for k in range(KC)] + ["fg"], w=[("ps", pu)])
                        P.ts("dve", Gc[:, 0:bn], ps[pg][:, 0:bn], bup[:, j:j + 1], SWL, op0=ALU.add, op1=ALU.min, r=[("ps", pg), "bup"], w=["Gc"])
                        P.actv(Sg[:, 0:bn], Gc[:, 0:bn], AF.Sigmoid, r=["Gc"], w=["Sg"], scale=SWA)
                        P.ts("dve", Uc[:, 0:bn], ps[pu][:, 0:bn], bup[:, 8 + j:9 + j], SWL, op0=ALU.add, op1=ALU.min, r=[("ps", pu), "bup"], w=["Uc"])
                        P.ts("pool", Uc[:, 0:bn], Uc[:, 0:bn], -SWL, 1.0, op0=ALU.max, op1=ALU.add, r=["Uc"], w=["Uc"])
                        P.tt("pool", Gc[:, 0:bn], Gc[:, 0:bn], Sg[:, 0:bn], ALU.mult, r=["Gc", "Sg"], w=["Gc"])
                        P.tt("pool", hb3[:, j, 0:bn], Gc[:, 0:bn], Uc[:, 0:bn], ALU.mult, r=["Gc", "Uc"], w=[("hid", bi % 2, j)])
                    for sub in range(bn // 128):
                        ti = (b0 + sub * 128) // 128
                        t = grp[ti]
                        for hf in range(2):
                            P.mm([(ps[4 + 2 * (sub % 2) + hf][:, 0:512], hb3[:, j, sub * 128:(sub + 1) * 128], wd3[:, j, hf * 512:(hf + 1) * 512], j == 0, j == KC - 1) for j in range(KC)],
                                 r=[("hid", bi % 2, j) for j in range(KC)] + [("wd", 0, k) for k in range(KC)], w=[("ps", 4 + 2 * (sub % 2) + hf)])
                            P.stt("dve", acc3[:, ti, hf * 512:(hf + 1) * 512], ps[4 + 2 * (sub % 2) + hf][:, 0:512], gates3[:, t, e:e + 1], acc3[:, ti, hf * 512:(hf + 1) * 512],
                                  ALU.mult, ALU.add, r=[("ps", 4 + 2 * (sub % 2) + hf), ("acc", ti, hf), ("gates", t)], w=[("acc", ti, hf)])
            for ti, t in enumerate(grp):
                w = 0 if t < NTL else 1
                P.dma("sp", xt, X[t * 128:(t + 1) * 128, :], r=[("X", t)], w=["xt"])
                P.tt("dve", acc3[:, ti, :], acc3[:, ti, :], GB[:, (2 + w) * D:(3 + w) * D], ALU.mult, r=[("acc", ti, 0), ("acc", ti, 1), "GB"], w=[("acc", ti, 0), ("acc", ti, 1)])
                P.tt("dve", xt, xt, acc3[:, ti, :], ALU.add, r=["xt", ("acc", ti, 0), ("acc", ti, 1)], w=["xt"])
                if final:
                    norm_tile(xt, sq, col)
                    P.stt("dve", xt, xt, col[:, 2:3], fn_, ALU.mult, ALU.mult, r=["xt", "col2", "fn"], w=["xt"])
                    P.dma("sp", out[t * 128:(t + 1) * 128, :], xt, r=["xt"], w=[("out", t)])
                else:
                    P.dma("sp", X[t * 128:(t + 1) * 128, :], xt, r=["xt"], w=[("X", t)])
        AR.reset(m)
        P.barrier()

    TOPK = 4
    SWL = 7.0
    SWA = 1.702
    for l in range(DEPTH):
        need_ctx = l < DEPTH - 1
        lam_init = 0.8 - 0.6 * math.exp(-0.3 * l)
        phase_mod(l)
        phase_inproj(l)
        if want("attn"):
            phase_attn(l, need_ctx, lam_init)
        if want("gmlp"):
            phase_gmlp(l, need_ctx)
        if want("gdn"):
            phase_gdn(l, need_ctx)
        phase_outproj(l, need_ctx)
        if want("ffn"):
            phase_ffn(l, need_ctx, l == DEPTH - 1)
    P.emit(st)
    st.close()
    return nc, P


def _consts(L):
    rows = L // 64
    r_idx, c_idx = np.meshgrid(np.arange(rows), np.arange(64), indexing="ij")
    rp = r_idx.reshape(-1).astype(np.float32)
    cp = c_idx.reshape(-1).astype(np.float32)

    def rope(dim):
        n = dim // 4
        inv = np.power(np.float32(10000.0), -np.arange(n, dtype=np.float32) / n).astype(np.float32)
        ang = np.concatenate([rp[:, None] * inv, cp[:, None] * inv], axis=-1).astype(np.float32)
        return np.concatenate([np.cos(ang), np.sin(ang)], axis=-1).astype(np.float32)

    idx = np.arange(64)
    gm = np.zeros((2, 64, 5, 64), np.float32)
    for dr in range(2):
        if dr == 0:
            U = (idx[:, None] <= idx[None, :])
            mS = (idx[:, None] > idx[None, :])
            mI = (idx[:, None] >= idx[None, :])
        else:
            U = (idx[:, None] >= idx[None, :])
            mS = (idx[:, None] < idx[None, :])
            mI = (idx[:, None] <= idx[None, :])
        gm[dr, :, 0, :] = U
        gm[dr, :, 1, :] = -mS.astype(np.float32)
        gm[dr, :, 2, :] = -mS.T.astype(np.float32)
        gm[dr, :, 3, :] = mI.T.astype(np.float32)
    return rope(64), rope(32), gm


def make_in_maps(inp, L, LC, DEPTH, NE, ncores):
    f = lambda a: np.ascontiguousarray(np.asarray(a, dtype=np.float32))
    ropa, ropd, gm = _consts(L)
    shared = {}
    shared["w_ada"] = f(inp["w_ada"])
    shared["b_ada"] = f(np.asarray(inp["b_ada"]).reshape(DEPTH, 48, 128).transpose(0, 2, 1))
    shared["nmix"] = f(np.asarray(inp["norm_mix"]).reshape(DEPTH, 8, 128).transpose(0, 2, 1))
    shared["nffn"] = f(np.asarray(inp["norm_ffn"]).reshape(DEPTH, 8, 128).transpose(0, 2, 1))
    shared["w_in"] = f(inp["w_in"])
    shared["w_out"] = f(inp["w_out"])
    gq = np.asarray(inp["gqa_q_norm"])
    gk = np.asarray(inp["gqa_k_norm"])
    shared["gqk"] = f(np.concatenate([np.tile(gq, (1, 4)), np.tile(gk, (1, 2))], axis=1).reshape(DEPTH, 1, 384))
    shared["gvn"] = f(np.asarray(inp["gmlp_v_norm"]).reshape(DEPTH, 1, 256))
    shared["w_s"] = f(inp["gmlp_w_s"])
    shared["b_s"] = f(np.asarray(inp["gmlp_b_s"]).transpose(0, 2, 1))
    shared["lam"] = f(np.concatenate([np.asarray(inp["diff_lambda_q1"]), np.asarray(inp["diff_lambda_k1"]),
                                      np.asarray(inp["diff_lambda_q2"]), np.asarray(inp["diff_lambda_k2"])], axis=1).reshape(DEPTH, 1, 128))
    shared["subln"] = f(np.asarray(inp["diff_subln"]).reshape(DEPTH, 1, 64))
    shared["convw"] = f(inp["dn_conv_w"])
    shared["alog"] = f(np.asarray(inp["dn_a_log"]).reshape(DEPTH, 1, 8))
    shared["dtb"] = f(np.asarray(inp["dn_dt_bias"]).reshape(DEPTH, 1, 8))
    shared["onorm"] = f(np.asarray(inp["dn_out_norm"]).reshape(DEPTH, 1, 64))
    shared["w_r"] = f(inp["router_w"])
    shared["b_r"] = f(np.asarray(inp["router_b"]).reshape(DEPTH, 1, NE))
    shared["w_up"] = f(inp["exp_w_up"])
    shared["b_up"] = f(np.asarray(inp["exp_b_up"]).reshape(DEPTH, NE, 16, 128).transpose(0, 1, 3, 2))
    shared["w_dn"] = f(inp["exp_w_down"])
    shared["b_dn"] = f(inp["exp_b_down"])
    shared["fnorm"] = f(np.asarray(inp["final_norm"]).reshape(1, D))
    shared["ropa"] = ropa
    shared["ropd"] = ropd
    shared["ident"] = np.eye(128, dtype=np.float32)
    shared["gmask"] = gm
    x = np.asarray(inp["x"])
    ctx = np.asarray(inp["ctx"])
    c = np.asarray(inp["c"])
    cc = np.asarray(inp["c_ctx"])
    maps = []
    for b in range(ncores):
        mp = dict(shared)
        mp["x0"] = f(np.concatenate([x[b], ctx[b]], axis=0))
        s = np.stack([c[b].reshape(8, 128).T, cc.reshape(8, 128).T], axis=-1)
        mp["scs"] = f(s.reshape(128, 16))
        maps.append(mp)
    return maps


_CACHE = {}


def kernel(**inputs):
    B, L, _ = inputs["x"].shape
    LC = inputs["ctx"].shape[1]
    DEPTH = inputs["w_in"].shape[0]
    NE = inputs["router_w"].shape[2]
    key = (L, LC, DEPTH, NE)
    if key not in _CACHE:
        _CACHE[key] = build(L, LC, DEPTH, NE)[0]
    nc = _CACHE[key]
    maps = make_in_maps(inputs, L, LC, DEPTH, NE, B)
    res = run_bass_kernel_spmd(nc, maps, core_ids=list(range(B)))
    return np.stack([res.results[b]["out"] for b in range(B)], axis=0).astype(np.float32)
```
